# Optimizing a Trainium2 kernel written in Bass

```python
import math
import jax, jax.numpy as jnp
from jax import lax
import numpy as np

D_MODEL = 1024
BATCH = 16
SEQ = 2048
DEPTH = 1

PLE_DIM = 256
SSM_WIDTH = 512
SSM_GROUP = 16
SSM_GROUPS = SSM_WIDTH // SSM_GROUP
SSM_STATE = 64
CONV_WIDTH = 512
CONV_GROUPS = 8
CONV_K = 3
N_EXPERT_GROUPS = 4
EXPERTS_PER_GROUP = 8
N_EXPERTS = N_EXPERT_GROUPS * EXPERTS_PER_GROUP
TOP_K_IN_GROUP = 2
EXPERT_HIDDEN = 256
PROJ_COLS = SSM_WIDTH + 3 * CONV_WIDTH + 2 * D_MODEL
NORM_EPS = 1e-6
DT_MIN = 1e-3
DT_MAX = 1e-1

kernel_name = "hybrid_s5_shortconv_hmoe_block"


def rms_norm(x, g):
    xf = x.astype(jnp.float32)
    y = xf * lax.rsqrt(jnp.mean(xf * xf, axis=-1, keepdims=True) + NORM_EPS)
    return (y * g.astype(jnp.float32)).astype(x.dtype)


def s5_mixer(u, lam_re, lam_im, log_dt, b_re, b_im, c_re, c_im, d_skip, w_glu_a, w_glu_b):
    f32 = jnp.float32
    bsz, seq, _ = u.shape
    uf = u.astype(f32).reshape(bsz, seq, SSM_GROUPS, SSM_GROUP)
    lr = lam_re.astype(f32)
    li = lam_im.astype(f32)
    dt = jnp.exp(log_dt.astype(f32))[:, None]
    mag = jnp.exp(lr * dt)
    ang = li * dt
    abar_re = mag * jnp.cos(ang)
    abar_im = mag * jnp.sin(ang)
    nr = abar_re - 1.0
    ni = abar_im
    den = lr * lr + li * li
    f_re = (nr * lr + ni * li) / den
    f_im = (ni * lr - nr * li) / den
    br = b_re.astype(f32)
    bi = b_im.astype(f32)
    bbar_re = f_re[:, :, None] * br - f_im[:, :, None] * bi
    bbar_im = f_re[:, :, None] * bi + f_im[:, :, None] * br
    x_re = jnp.einsum('blgh,gph->blgp', uf, bbar_re)
    x_im = jnp.einsum('blgh,gph->blgp', uf, bbar_im)
    a_re = jnp.broadcast_to(abar_re, (1, seq, SSM_GROUPS, SSM_STATE))
    a_im = jnp.broadcast_to(abar_im, (1, seq, SSM_GROUPS, SSM_STATE))

    def combine(e_i, e_j):
        ar_i, ai_i, sr_i, si_i = e_i
        ar_j, ai_j, sr_j, si_j = e_j
        return (ar_j * ar_i - ai_j * ai_i,
                ar_j * ai_i + ai_j * ar_i,
                ar_j * sr_i - ai_j * si_i + sr_j,
                ar_j * si_i + ai_j * sr_i + si_j)

    _, _, s_re, s_im = lax.associative_scan(combine, (a_re, a_im, x_re, x_im), axis=1)
    y = (jnp.einsum('blgp,ghp->blgh', s_re, c_re.astype(f32))
         - jnp.einsum('blgp,ghp->blgh', s_im, c_im.astype(f32)))
    y = y.reshape(bsz, seq, SSM_WIDTH) + d_skip.astype(f32) * uf.reshape(bsz, seq, SSM_WIDTH)
    y = jax.nn.gelu(y).astype(u.dtype)
    return (y @ w_glu_a) * jax.nn.sigmoid(y @ w_glu_b)


def short_conv_mixer(gate_b, gate_c, v, conv_w, conv_b, w_out):
    cv = gate_c * v
    y = lax.conv_general_dilated(
        cv, conv_w[:, None, :], window_strides=(1,), padding=[(CONV_K - 1, 0)],
        dimension_numbers=('NWC', 'WIO', 'NWC'), feature_group_count=CONV_WIDTH)
    y = y + conv_b
    return (gate_b * y) @ w_out


def hier_moe(h, w_rg, b_rg, w_re, b_re, w_gate, w_up, w_down):
    f32 = jnp.float32
    bsz, seq, d = h.shape
    t = h.reshape(-1, d)
    p_group = jax.nn.softmax((t @ w_rg + b_rg).astype(f32), axis=-1)
    g_w, g_idx = lax.top_k(p_group, 1)
    le = (t @ w_re + b_re).astype(f32).reshape(-1, N_EXPERT_GROUPS, EXPERTS_PER_GROUP)
    le_sel = jnp.take_along_axis(le, g_idx[:, :, None], axis=1)[:, 0]
    p_exp = jax.nn.softmax(le_sel, axis=-1)
    e_w, e_idx = lax.top_k(p_exp, TOP_K_IN_GROUP)
    e_w = e_w / jnp.sum(e_w, axis=-1, keepdims=True)
    weights = g_w * e_w
    ids = g_idx * EXPERTS_PER_GROUP + e_idx
    comb = jnp.sum(jax.nn.one_hot(ids, N_EXPERTS, dtype=f32) * weights[..., None], axis=1)
    out = jnp.zeros(t.shape, f32)
    for e in range(N_EXPERTS):
        hid = jax.nn.silu(t @ w_gate[e]) * (t @ w_up[e])
        out = out + comb[:, e:e + 1] * (hid @ w_down[e]).astype(f32)
    return out.reshape(bsz, seq, d).astype(h.dtype)


def setup_inputs(seed: int = 0) -> dict:
    key = jax.random.key(seed)
    ks = jax.random.split(key, 32)
    f32 = jnp.float32

    def dense(k, shape, fan_in):
        return jax.random.normal(k, shape, f32) * (fan_in ** -0.5)

    def gain(k, shape):
        return 1.0 + 0.01 * jax.random.normal(k, shape, f32)

    L = DEPTH
    lam_im0 = math.pi * jnp.arange(SSM_STATE, dtype=f32)
    return {
        "x": jax.random.normal(ks[0], (BATCH, SEQ, D_MODEL), f32),
        "p": jax.random.normal(ks[1], (DEPTH, BATCH, SEQ, PLE_DIM), f32),
        "norm_mix": gain(ks[2], (L, D_MODEL)),
        "w_in": dense(ks[3], (L, D_MODEL, PROJ_COLS), D_MODEL),
        "b_in": 0.01 * jax.random.normal(ks[4], (L, PROJ_COLS), f32),
        "ssm_lam_re": -0.5 + 0.01 * jax.random.normal(ks[5], (L, SSM_GROUPS, SSM_STATE), f32),
        "ssm_lam_im": lam_im0 + 0.01 * jax.random.normal(ks[6], (L, SSM_GROUPS, SSM_STATE), f32),
        "ssm_log_dt": jax.random.uniform(ks[7], (L, SSM_GROUPS), f32, math.log(DT_MIN), math.log(DT_MAX)),
        "ssm_b_re": dense(ks[8], (L, SSM_GROUPS, SSM_STATE, SSM_GROUP), 2 * SSM_GROUP),
        "ssm_b_im": dense(ks[9], (L, SSM_GROUPS, SSM_STATE, SSM_GROUP), 2 * SSM_GROUP),
        "ssm_c_re": dense(ks[10], (L, SSM_GROUPS, SSM_GROUP, SSM_STATE), SSM_STATE),
        "ssm_c_im": dense(ks[11], (L, SSM_GROUPS, SSM_GROUP, SSM_STATE), SSM_STATE),
        "ssm_d": jax.random.normal(ks[12], (L, SSM_WIDTH), f32),
        "w_glu_a": dense(ks[13], (L, SSM_WIDTH, D_MODEL), SSM_WIDTH),
        "w_glu_b": dense(ks[14], (L, SSM_WIDTH, D_MODEL), SSM_WIDTH),
        "conv_w": dense(ks[15], (L, CONV_K, CONV_WIDTH), CONV_K),
        "conv_b": 0.01 * jax.random.normal(ks[16], (L, CONV_WIDTH), f32),
        "w_conv_out": dense(ks[17], (L, CONV_WIDTH, D_MODEL), CONV_WIDTH),
        "w_o": dense(ks[18], (L, D_MODEL, D_MODEL), D_MODEL),
        "norm_ffn": gain(ks[19], (L, D_MODEL)),
        "w_router_group": dense(ks[20], (L, D_MODEL, N_EXPERT_GROUPS), D_MODEL),
        "b_router_group": 0.01 * jax.random.normal(ks[21], (L, N_EXPERT_GROUPS), f32),
        "w_router_expert": dense(ks[22], (L, D_MODEL, N_EXPERTS), D_MODEL),
        "b_router_expert": 0.01 * jax.random.normal(ks[23], (L, N_EXPERTS), f32),
        "w_exp_gate": dense(ks[24], (L, N_EXPERTS, D_MODEL, EXPERT_HIDDEN), D_MODEL),
        "w_exp_up": dense(ks[25], (L, N_EXPERTS, D_MODEL, EXPERT_HIDDEN), D_MODEL),
        "w_exp_down": dense(ks[26], (L, N_EXPERTS, EXPERT_HIDDEN, D_MODEL), EXPERT_HIDDEN),
        "norm_ple": gain(ks[27], (L, D_MODEL)),
        "w_ple": dense(ks[28], (L, PLE_DIM, D_MODEL), PLE_DIM),
        "w_ple_gate": dense(ks[29], (L, D_MODEL, D_MODEL), D_MODEL),
        "b_ple_gate": 0.01 * jax.random.normal(ks[30], (L, D_MODEL), f32),
        "norm_final": gain(ks[31], (D_MODEL,)),
    }


def reference(x, p, norm_mix, w_in, b_in, ssm_lam_re, ssm_lam_im, ssm_log_dt,
              ssm_b_re, ssm_b_im, ssm_c_re, ssm_c_im, ssm_d, w_glu_a, w_glu_b,
              conv_w, conv_b, w_conv_out, w_o, norm_ffn, w_router_group,
              b_router_group, w_router_expert, b_router_expert, w_exp_gate,
              w_exp_up, w_exp_down, norm_ple, w_ple, w_ple_gate, b_ple_gate,
              norm_final):
    s0 = SSM_WIDTH
    s1 = s0 + CONV_WIDTH
    s2 = s1 + CONV_WIDTH
    s3 = s2 + CONV_WIDTH
    s4 = s3 + D_MODEL
    for i in range(DEPTH):
        h = rms_norm(x, norm_mix[i])
        z = h @ w_in[i] + b_in[i]
        u_ssm = z[..., :s0]
        c_b, c_c, c_v = z[..., s0:s1], z[..., s1:s2], z[..., s2:s3]
        g_a = jax.nn.sigmoid(z[..., s3:s4])
        g_b = jax.nn.sigmoid(z[..., s4:])
        y_a = s5_mixer(u_ssm, ssm_lam_re[i], ssm_lam_im[i], ssm_log_dt[i],
                       ssm_b_re[i], ssm_b_im[i], ssm_c_re[i], ssm_c_im[i],
                       ssm_d[i], w_glu_a[i], w_glu_b[i])
        y_b = short_conv_mixer(c_b, c_c, c_v, conv_w[i], conv_b[i], w_conv_out[i])
        x = x + (g_a * y_a + g_b * y_b) @ w_o[i]
        h2 = rms_norm(x, norm_ffn[i])
        x = x + hier_moe(h2, w_router_group[i], b_router_group[i],
                         w_router_expert[i], b_router_expert[i],
                         w_exp_gate[i], w_exp_up[i], w_exp_down[i])
        h3 = rms_norm(x, norm_ple[i])
        gate = jax.nn.sigmoid(h3 @ w_ple_gate[i] + b_ple_gate[i])
        x = x + gate * (p[i] @ w_ple[i])
    return rms_norm(x, norm_final)
```

```python
import math
from contextlib import ExitStack

import numpy as np
import concourse.bass as bass
import concourse.mybir as mybir
from concourse.bass_utils import run_bass_kernel_spmd

F32 = mybir.dt.float32
BF16 = mybir.dt.bfloat16
I32 = mybir.dt.int32
AF = mybir.ActivationFunctionType
ALU = mybir.AluOpType
AX = mybir.AxisListType

NCORES = 8
SEQ = 2048
TOK = 4096
D = 1024
TB = 512
NBLK = TOK // TB
CH = 128
NS = 8
DEPTH = 4
PI = math.pi
BIG = 1.0e9
SAME_ENGINE_FREE = False


class Op:
    __slots__ = ("eng", "fn", "reads", "writes", "dma_key", "deps", "inc", "cnt", "waits", "dma_val")

    def __init__(self, eng, fn, reads, writes, dma_key):
        self.eng = eng
        self.fn = fn
        self.reads = tuple(reads)
        self.writes = tuple(writes)
        self.dma_key = dma_key
        self.inc = False
        self.cnt = 0
        self.waits = []
        self.dma_val = 0


class Prog:
    ENG = ("pe", "act", "dve", "pool", "sp")

    def __init__(self, nc, es):
        self.nc = nc
        self.es = es
        self.ops = []

    def sb(self, name, shape, dtype):
        self.sbytes = getattr(self, "sbytes", 0) + int(np.prod(shape[1:])) * mybir.dt.size(dtype)
        return self.es.enter_context(self.nc.sbuf_tensor(name, list(shape), dtype))

    def ps(self, name, shape, dtype):
        return self.es.enter_context(self.nc.psum_tensor(name, list(shape), dtype))

    def add(self, eng, fn, reads=(), writes=(), dma_key=None):
        self.ops.append(Op(eng, fn, reads, writes, dma_key))

    def pe(self, fn, reads=(), writes=()):
        self.add("pe", fn, reads, writes)

    def act(self, fn, reads=(), writes=()):
        self.add("act", fn, reads, writes)

    def dve(self, fn, reads=(), writes=()):
        self.add("dve", fn, reads, writes)

    def pool(self, fn, reads=(), writes=()):
        self.add("pool", fn, reads, writes)

    def dma(self, eng, out, in_, reads=(), writes=(), key=None, **kw):
        assert key is not None
        self.add(eng, lambda e: e.dma_start(out=out, in_=in_, **kw), reads, writes, dma_key=key)

    def finalize(self):
        nc, es, ops = self.nc, self.es, self.ops
        last_w, readers, dma_cnt = {}, {}, {}
        for i, op in enumerate(ops):
            deps = set()
            for r in op.reads:
                if r in last_w:
                    deps.add(last_w[r])
            for w in op.writes:
                if w in last_w:
                    deps.add(last_w[w])
                deps.update(readers.get(w, ()))
            deps.discard(i)
            op.deps = deps
            for r in op.reads:
                readers.setdefault(r, []).append(i)
            for w in op.writes:
                last_w[w] = i
                readers[w] = []
            if op.dma_key is not None:
                dma_cnt[op.dma_key] = dma_cnt.get(op.dma_key, 0) + 1
                op.dma_val = 16 * dma_cnt[op.dma_key]

        def skip(pj, op):
            if pj.eng == "pe" and op.eng == "pe":
                return True
            return SAME_ENGINE_FREE and pj.eng == op.eng and pj.eng in ("dve",) and op.dma_key is None

        for op in ops:
            for j in op.deps:
                pj = ops[j]
                if pj.dma_key is None and not skip(pj, op):
                    pj.inc = True
        cnt = {e: 0 for e in self.ENG}
        for op in ops:
            if op.inc:
                cnt[op.eng] += 1
                op.cnt = cnt[op.eng]
        self.ndma = len(dma_cnt)
        print("n dma keys", len(dma_cnt), "sbuf KiB/partition", self.sbytes / 1024)
        esem = {e: es.enter_context(nc.semaphore("s_" + e)) for e in self.ENG}
        dsem = {}
        for k in dma_cnt:
            dsem[k] = es.enter_context(nc.semaphore("d%d" % len(dsem)))
        self.nsem = len(esem) + len(dsem)
        waited = {e: {} for e in self.ENG}
        for op in ops:
            w = {}
            for j in op.deps:
                pj = ops[j]
                if pj.dma_key is not None:
                    sid, val = ("d", pj.dma_key), pj.dma_val
                else:
                    if skip(pj, op):
                        continue
                    sid, val = ("e", pj.eng), pj.cnt
                if val > w.get(sid, 0):
                    w[sid] = val
            wd = waited[op.eng]
            for sid, val in w.items():
                if val > wd.get(sid, 0):
                    wd[sid] = val
                    sem = dsem[sid[1]] if sid[0] == "d" else esem[sid[1]]
                    op.waits.append((sem, val))
        per = {e: [op for op in ops if op.eng == e] for e in self.ENG}
        self.counts = {e: len(per[e]) for e in self.ENG}
        self.maxcnt = dict(cnt)

        def replay(name, eng):
            for op in per[name]:
                for sem, val in op.waits:
                    eng.wait_ge(sem, val)
                if op.fn is None:
                    continue
                inst = op.fn(eng)
                if op.dma_key is not None:
                    inst.then_inc(dsem[op.dma_key], 16)
                elif op.inc:
                    inst.then_inc(esem[name], 1)

        with nc.Block() as block:
            @block.tensor
            def _(e):
                replay("pe", e)

            @block.scalar
            def _(e):
                replay("act", e)

            @block.vector
            def _(e):
                replay("dve", e)

            @block.gpsimd
            def _(e):
                replay("pool", e)

            @block.sync
            def _(e):
                replay("sp", e)


class PsumAlloc:
    def __init__(self, P):
        self.t = [P.ps("psb%d" % i, [128, 512], F32) for i in range(8)]
        self.free_list = list(range(8))

    def alloc(self):
        assert self.free_list, "PSUM exhausted"
        return self.free_list.pop(0)

    def free(self, b):
        assert b not in self.free_list
        self.free_list.append(b)


class WStream:
    def __init__(self, P, ring, order):
        self.P = P
        self.ring = ring
        self.order = order
        self.next_dma = 0
        self.next_get = 0
        self.dead = -1

    def slot_view(self, k, shape):
        s = k % NS
        v = self.ring[:, s * 2048:(s + 1) * 2048]
        if len(shape) == 2:
            return v.rearrange("p (a b) -> p a b", a=shape[0])
        return v

    def get(self, tag):
        k = self.next_get
        assert self.order[k][0] == tag, (self.order[k][0], tag)
        self.next_get += 1
        lim = min(len(self.order), k + DEPTH + 1)
        while self.next_dma < lim:
            j = self.next_dma
            _, src, shape, rkeys = self.order[j]
            self.P.dma("sp", self.slot_view(j, shape), src, reads=rkeys, writes=[("w", j % NS)], key=("w", j % NS))
            self.dead = max(self.dead, j - NS)
            self.next_dma += 1
        return (k, self.slot_view(k, self.order[k][2]), ("w", k % NS))

    def check(self, h):
        assert h[0] > self.dead, "weight chunk overwritten before use"


def build_program(nblk=NBLK, stop="full"):
    assert stop == "full"
    nc = bass.Bass("TRN2", target_bir_lowering=False)

    def din(name, shape):
        return nc.dram_tensor(name, list(shape), F32, kind="ExternalInput").ap()

    def dscr(name, shape):
        return nc.dram_tensor(name, list(shape), BF16, kind="Internal").ap()

    x_d = din("x", [TOK, D])
    p_d = din("p", [TOK, 256])
    norm_mix = din("norm_mix", [D])
    w_in = din("w_in", [D, 4096])
    b_in = din("b_in", [4096])
    lam_re = din("ssm_lam_re", [32, 64])
    lam_im = din("ssm_lam_im", [32, 64])
    log_dt = din("ssm_log_dt", [32])
    sb_re = din("ssm_b_re", [32, 64, 16])
    sb_im = din("ssm_b_im", [32, 64, 16])
    sc_re = din("ssm_c_re", [32, 16, 64])
    sc_im = din("ssm_c_im", [32, 16, 64])
    ssm_d = din("ssm_d", [512])
    w_glu_a = din("w_glu_a", [512, D])
    w_glu_b = din("w_glu_b", [512, D])
    conv_w = din("conv_w", [3, 512])
    conv_b = din("conv_b", [512])
    w_conv_out = din("w_conv_out", [512, D])
    w_o = din("w_o", [D, D])
    norm_ffn = din("norm_ffn", [D])
    w_rg = din("w_router_group", [D, 4])
    b_rg = din("b_router_group", [4])
    w_re = din("w_router_expert", [D, 32])
    b_re = din("b_router_expert", [32])
    w_eg = din("w_exp_gate", [32, D, 256])
    w_eu = din("w_exp_up", [32, D, 256])
    w_ed = din("w_exp_down", [32, 256, D])
    norm_ple = din("norm_ple", [D])
    w_ple = din("w_ple", [256, D])
    w_pg = din("w_ple_gate", [D, D])
    b_pg = din("b_ple_gate", [D])
    norm_final = din("norm_final", [D])
    y_d = nc.dram_tensor("y", [TOK, D], F32, kind="ExternalOutput").ap()

    win_s = dscr("win_s", [D, 4096])
    glua_s = dscr("glua_s", [512, D])
    glub_s = dscr("glub_s", [512, D])
    wco_s = dscr("wco_s", [512, D])
    wo_s = dscr("wo_s", [D, D])
    wpg_s = dscr("wpg_s", [D, D])
    wple_s = dscr("wple_s", [256, D])
    wgp = dscr("wgp", [32 * 128, 2048])
    wup = dscr("wup", [32 * 128, 2048])
    wdp = dscr("wdp", [32 * 128, 2048])
    h2_s = dscr("h2_s", [TOK, D])
    hs = dscr("hs", [96 * 128, D])
    x1_s = nc.dram_tensor("x1_s", [TOK, D], F32, kind="Internal").ap()
    ys = nc.dram_tensor("ys", [96 * 128, D], F32, kind="Internal").ap()

    with ExitStack() as es:
        P = Prog(nc, es)
        PS = PsumAlloc(P)
        ps = PS.t

        def psk(b):
            return ("ps", b)

        xres = P.sb("xres", [128, 4, D], F32)
        hT = P.sb("hT", [128, 8, TB], BF16)
        hn = P.sb("hn", [128, 2, D], BF16)
        stat = P.sb("stat", [128, 16], F32)
        uT = P.sb("uT", [128, 4, TB], BF16)
        s5t = P.sb("s5t", [128, 4, TB], F32)
        sri = P.sb("sri", [128, 2, 2, TB], BF16)
        wlast = P.sb("wlast", [128, 16, 2], F32)
        chn = P.sb("chn", [128, 2, 4], F32)
        s5u = P.sb("s5u", [128, 4, TB], F32)
        s5p = P.sb("s5p", [128, 2, TB], F32)
        COS = P.sb("COS", [128, 16, CH], F32)
        SIN = P.sb("SIN", [128, 16, CH], F32)
        LBk = [P.sb("LBk%d" % i, [128, 4, 4, 128], BF16) for i in range(2)]
        LCk = [P.sb("LCk%d" % i, [128, 4, 16, 32], BF16) for i in range(2)]
        diagD = P.sb("diagD", [128, 4, 128], BF16)
        cols = P.sb("cols", [128, 108], F32)
        stg = P.sb("stg", [128, 128], F32)
        sc = P.sb("sc", [128, 24, 16], F32)
        bnat = [P.sb("bnat%d" % i, [128, 16, 16], F32) for i in range(2)]
        btmp = [P.sb("btmp%d" % i, [128, 16, 16], F32) for i in range(4)]
        cnat = [P.sb("cnat%d" % i, [128, 4, 64], F32) for i in range(2)]
        cmk = P.sb("cmk", [128, 4, 8], F32)
        ybf = P.sb("ybf", [128, 4, TB], BF16)
        arena = P.sb("arena", [128, 4112], F32)
        abuf = arena[:, 0:2048].rearrange("p (a b) -> p a b", a=4)
        cvbuf = arena[:, 2048:2048 + 4 * (TB + 2)].rearrange("p (a b) -> p a b", a=4)
        gbT = P.sb("gbT", [128, 4, TB], BF16)
        mixT = P.sb("mixT", [128, 8, TB], BF16)
        lsb = P.sb("lsb", [128, 4, 36], F32)
        brB = P.sb("brB", [128, 36], F32)
        wr = P.sb("wr", [128, 8, 36], BF16)
        pb = P.sb("pb", [128, 4, 256], BF16)
        pT = P.sb("pT", [128, 2, TB], BF16)
        bpgB = P.sb("bpgB", [128, D], F32)
        gfinB = P.sb("gfinB", [128, D], F32)
        ost = P.sb("ost", [128, D], F32)
        junk = ost[:].bitcast(BF16)[:, 0:D]
        idf = P.sb("idf", [128, 128], F32)
        idb = P.sb("idb", [128, 128], BF16)
        idi = P.sb("idi", [128, 128], F32)
        ring = P.sb("ring", [128, NS * 2048], BF16)

        XK = [("x", j) for j in range(4)]
        S5K = [("s5t", i) for i in range(4)]
        CVK = [("cv", m) for m in range(4)]

        def flat2(ap, rows):
            return ap.rearrange("(a b) c -> a (b c)", a=rows)

        for cc in (0, 2, 3, 1, 4, 5, 6, 7):
            P.dma("pool", win_s[:, cc * 512:(cc + 1) * 512], w_in[:, cc * 512:(cc + 1) * 512], writes=[("win_s", cc)], key=("win_s", cc))
        for nm, dst, src in (("glua_s", glua_s, w_glu_a), ("glub_s", glub_s, w_glu_b), ("wco_s", wco_s, w_conv_out),
                             ("wo_s", wo_s, w_o)):
            P.dma("pool", flat2(dst, 128), flat2(src, 128), writes=[nm], key=nm)
        prepass_q = []

        def emit_prepass(n):
            for _ in range(min(n, len(prepass_q))):
                d_, s_, wk_, key_ = prepass_q.pop(0)
                P.dma("pool", d_, s_, writes=[wk_], key=key_)

        for i, (dst, src, kk) in enumerate(((wgp, w_eg, 8), (wup, w_eu, 8), (wdp, w_ed, 2))):
            for g in range(4):
                d4 = dst[g * 1024:(g + 1) * 1024, :].rearrange("(e p) (k c) -> e k p c", p=128, k=kk)
                s4 = src[8 * g:8 * g + 8].rearrange("e (k p) c -> e k p c", p=128)
                for e8 in range(8):
                    prepass_q.append((d4[e8], s4[e8], ("wpermw", i, g, e8), ("wperm", i)))
        prepass_q.sort(key=lambda q: (q[2][2], q[2][3], q[2][1]))
        WPK = {i: [("wpermw", i, g, e8) for g in range(4) for e8 in range(8)] for i in range(3)}
        for nm, dst, src in (("wpg_s", wpg_s, w_pg), ("wple_s", wple_s, w_ple)):
            P.dma("pool", flat2(dst, 128), flat2(src, 128), writes=[nm], key=nm)

        def kt_view(ap2d):
            return ap2d.rearrange("(kt p) n -> p kt n", p=128)

        order = []
        for blk in range(nblk):
            def win(c):
                order.append((("win", c), kt_view(win_s)[:, :, c * 256:(c + 1) * 256], (8, 256), [("win_s", c // 2)]))
            for c in (0, 1, 4, 5, 6, 7, 2, 3):
                win(c)
            for h in range(2):
                order.append((("glua", h), kt_view(glua_s)[:, :, h * 512:(h + 1) * 512], (4, 512), ["glua_s"]))
                order.append((("glub", h), kt_view(glub_s)[:, :, h * 512:(h + 1) * 512], (4, 512), ["glub_s"]))
                win(8 + 2 * h)
                win(9 + 2 * h)
            for h in range(2):
                order.append((("wco", h), kt_view(wco_s)[:, :, h * 512:(h + 1) * 512], (4, 512), ["wco_s"]))
                win(12 + 2 * h)
                win(13 + 2 * h)
            for hf in range(2):
                for kh in range(2):
                    order.append((("wo", hf, kh), kt_view(wo_s)[:, 4 * kh:4 * kh + 4, hf * 512:(hf + 1) * 512], (4, 512), ["wo_s"]))
        for hf in range(2):
            for kh in range(2):
                order.append((("wpg", hf, kh), kt_view(wpg_s)[:, 4 * kh:4 * kh + 4, hf * 512:(hf + 1) * 512], (4, 512), ["wpg_s"]))
            if hf == 0:
                order.append((("wple",), kt_view(wple_s), (2, 1024), ["wple_s"]))
        WS = WStream(P, ring, order)

        P.pool(lambda e: e.iota(idi[:], pattern=[[1, 128]], base=0, channel_multiplier=-1,
                                allow_small_or_imprecise_dtypes=True), writes=["idi"])
        P.dve(lambda e: e.tensor_single_scalar(out=idf[:], in_=idi[:], scalar=0.0, op=ALU.is_equal), reads=["idi"], writes=["idf"])
        P.dve(lambda e: e.tensor_copy(out=idb[:], in_=idf[:]), reads=["idf"], writes=["idb"])
        P.pool(lambda e: e.iota(cmk[:, 0, :], pattern=[[-16, 8]], base=0, channel_multiplier=1,
                                allow_small_or_imprecise_dtypes=True), writes=["cmk0"])
        P.dve(lambda e: e.tensor_single_scalar(out=cmk[:, 1, :], in_=cmk[:, 0, :], scalar=0.0, op=ALU.is_ge), reads=["cmk0"], writes=["cmk1"])
        P.dve(lambda e: e.tensor_single_scalar(out=cmk[:, 2, :], in_=cmk[:, 0, :], scalar=15.0, op=ALU.is_le), reads=["cmk0"], writes=["cmk2"])
        P.dve(lambda e: e.tensor_tensor(out=cmk[:, 2, :], in0=cmk[:, 2, :], in1=cmk[:, 1, :], op=ALU.mult), reads=["cmk1", "cmk2"], writes=["cmk2"])
        P.dve(lambda e: e.tensor_scalar(out=cmk[:, 3, :], in0=cmk[:, 2, :], scalar1=-1.0, scalar2=None, op0=ALU.mult), reads=["cmk2"], writes=["cmk3"])

        P.dve(lambda e: e.memset(stg[:], 0.0), writes=["stg"])
        rows = [(norm_mix, 0, 8), (norm_ffn, 8, 8), (norm_ple, 16, 8), (b_in, 24, 32), (ssm_d, 56, 4),
                (conv_w.rearrange("k c -> (k c)"), 60, 12), (conv_b, 72, 4),
                (lam_re.rearrange("g p -> (g p)"), 76, 16), (lam_im.rearrange("g p -> (g p)"), 92, 16)]
        for i, (src, r0, n) in enumerate(rows):
            P.dma("sp", stg[r0:r0 + n, :], src.rearrange("(a b) -> a b", b=128), reads=["stg"], writes=[("stgr", i)], key=("stgr", i))
        b0 = PS.alloc()
        P.pe(lambda e: e.transpose(out=ps[b0][:, 0:128], in_=stg[:, :], identity=idf[:]),
             reads=["idf", "stg"] + [("stgr", i) for i in range(len(rows))], writes=[psk(b0)])
        P.dve(lambda e: e.tensor_copy(out=cols[:], in_=ps[b0][:, 0:108]), reads=[psk(b0)], writes=["cols"])
        PS.free(b0)
        gmix, gffn, gple = cols[:, 0:8], cols[:, 8:16], cols[:, 16:24]
        binc, dcol, cw, cbc = cols[:, 24:56], cols[:, 56:60], cols[:, 60:72], cols[:, 72:76]
        lre, lim = cols[:, 76:92], cols[:, 92:108]

        P.dma("sp", bpgB[:], b_pg.partition_broadcast(128), writes=["bpgB"], key="bpgB")
        P.dma("sp", gfinB[:], norm_final.partition_broadcast(128), writes=["gfinB"], key="gfinB")
        P.dma("sp", brB[:, 0:4], b_rg.partition_broadcast(128), writes=["brB0"], key="brB0")
        P.dma("sp", brB[:, 4:36], b_re.partition_broadcast(128), writes=["brB1"], key="brB1")
        BRB = ["brB0", "brB1"]
        P.dma("pool", wr[:, :, 0:4], kt_view(w_rg), writes=["wr0"], key="wr0")
        P.dma("pool", wr[:, :, 4:36], kt_view(w_re), writes=["wr1"], key="wr1")
        WRK = ["wr0", "wr1"]

        def S(i):
            return sc[:, i, :]

        def sk(i):
            return ("sc", i)

        ldv = log_dt.rearrange("(s g) -> g s", g=2)
        for g2 in range(2):
            P.dma("sp", sc[g2 * 64:(g2 + 1) * 64, 0, :], ldv[g2].partition_broadcast(64), writes=[("ldt", g2)],
                  key=("ldt", g2), allow_slow_non_contiguous=True)
        LDT = [("ldt", 0), ("ldt", 1)]

        def tt(out, a, b, op, reads, writes):
            P.dve(lambda e: e.tensor_tensor(out=out, in0=a, in1=b, op=op), reads=reads, writes=writes)

        def wrap(ap, tmp, rk, tk, n=1):
            for _ in range(n):
                P.dve(lambda e: e.tensor_scalar(out=tmp, in0=ap, scalar1=PI, scalar2=2 * PI, op0=ALU.is_gt, op1=ALU.mult), reads=rk, writes=tk)
                P.dve(lambda e: e.tensor_tensor(out=ap, in0=ap, in1=tmp, op=ALU.subtract), reads=rk + tk, writes=rk)
                P.dve(lambda e: e.tensor_scalar(out=tmp, in0=ap, scalar1=-PI, scalar2=2 * PI, op0=ALU.is_lt, op1=ALU.mult), reads=rk, writes=tk)
                P.dve(lambda e: e.tensor_tensor(out=ap, in0=ap, in1=tmp, op=ALU.add), reads=rk + tk, writes=rk)

        P.act(lambda e: e.activation(out=S(0), in_=S(0), func=AF.Exp), reads=LDT, writes=[sk(0)])
        tt(S(1), lre, S(0), ALU.mult, ["cols", sk(0)], [sk(1)])
        P.act(lambda e: e.activation(out=S(1), in_=S(1), func=AF.Exp), reads=[sk(1)], writes=[sk(1)])
        tt(S(2), lim, S(0), ALU.mult, ["cols", sk(0)], [sk(2)])
        wrap(S(2), S(10), [sk(2)], [sk(10)], n=8)
        P.act(lambda e: e.activation(out=S(3), in_=S(2), func=AF.Sin), reads=[sk(2)], writes=[sk(3)])
        P.dve(lambda e: e.tensor_scalar(out=S(11), in0=S(2), scalar1=PI / 2, scalar2=None, op0=ALU.add), reads=[sk(2)], writes=[sk(11)])
        wrap(S(11), S(10), [sk(11)], [sk(10)])
        P.act(lambda e: e.activation(out=S(4), in_=S(11), func=AF.Sin), reads=[sk(11)], writes=[sk(4)])
        tt(S(5), S(1), S(4), ALU.mult, [sk(1), sk(4)], [sk(5)])
        tt(S(6), S(1), S(3), ALU.mult, [sk(1), sk(3)], [sk(6)])
        P.dve(lambda e: e.tensor_scalar(out=S(5), in0=S(5), scalar1=-1.0, scalar2=None, op0=ALU.add), reads=[sk(5)], writes=[sk(5)])
        tt(S(7), lre, lre, ALU.mult, ["cols"], [sk(7)])
        tt(S(10), lim, lim, ALU.mult, ["cols"], [sk(10)])
        tt(S(7), S(7), S(10), ALU.add, [sk(7), sk(10)], [sk(7)])
        P.dve(lambda e: e.reciprocal(out=S(7), in_=S(7)), reads=[sk(7)], writes=[sk(7)])
        tt(S(8), S(5), lre, ALU.mult, [sk(5), "cols"], [sk(8)])
        tt(S(10), S(6), lim, ALU.mult, [sk(6), "cols"], [sk(10)])
        tt(S(8), S(8), S(10), ALU.add, [sk(8), sk(10)], [sk(8)])
        tt(S(8), S(8), S(7), ALU.mult, [sk(8), sk(7)], [sk(8)])
        tt(S(9), S(6), lre, ALU.mult, [sk(6), "cols"], [sk(9)])
        tt(S(10), S(5), lim, ALU.mult, [sk(5), "cols"], [sk(10)])
        tt(S(9), S(9), S(10), ALU.subtract, [sk(9), sk(10)], [sk(9)])
        tt(S(9), S(9), S(7), ALU.mult, [sk(9), sk(7)], [sk(9)])

        xflat = xres[:].rearrange("p a b -> p (a b)")
        ANG = xflat[:, 0:CH * 16]
        TMPW = xflat[:, CH * 16:2 * CH * 16]
        s5flat = s5t[:].rearrange("p a b -> p (a b)")
        P.dve(lambda e: e.memset(ANG[:, 0:16], 0.0), writes=XK)
        P.dve(lambda e: e.tensor_copy(out=S(12), in_=S(2)), reads=[sk(2)], writes=[sk(12)])
        n = 1
        while n < CH:
            dst = ANG[:, n * 16:2 * n * 16]
            src = ANG[:, 0:n * 16]
            P.dve(lambda e, dst=dst, src=src, n=n: e.tensor_tensor(
                out=dst.rearrange("p (j s) -> p j s", s=16), in0=src.rearrange("p (j s) -> p j s", s=16),
                in1=S(12).unsqueeze(1).to_broadcast([128, n, 16]), op=ALU.add), reads=XK + [sk(12)], writes=XK)
            wrap(dst, s5flat[:, 0:n * 16], XK, S5K)
            P.dve(lambda e: e.tensor_scalar(out=S(12), in0=S(12), scalar1=2.0, scalar2=None, op0=ALU.mult), reads=[sk(12)], writes=[sk(12)])
            wrap(S(12), S(10), [sk(12)], [sk(10)])
            n *= 2
        P.dve(lambda e: e.tensor_scalar(out=S(15), in0=S(12), scalar1=2.0, scalar2=None, op0=ALU.mult), reads=[sk(12)], writes=[sk(15)])
        wrap(S(15), S(10), [sk(15)], [sk(10)])
        tt(S(16), S(15), S(12), ALU.add, [sk(15), sk(12)], [sk(16)])
        wrap(S(16), S(10), [sk(16)], [sk(10)])
        P.dve(lambda e: e.tensor_scalar(out=S(17), in0=S(15), scalar1=2.0, scalar2=None, op0=ALU.mult), reads=[sk(15)], writes=[sk(17)])
        wrap(S(17), S(10), [sk(17)], [sk(10)])

        def cossin(pi_, ci_, si_):
            P.act(lambda e: e.activation(out=S(si_), in_=S(pi_), func=AF.Sin), reads=[sk(pi_)], writes=[sk(si_)])
            P.dve(lambda e: e.tensor_scalar(out=S(11), in0=S(pi_), scalar1=PI / 2, scalar2=None, op0=ALU.add), reads=[sk(pi_)], writes=[sk(11)])
            wrap(S(11), S(10), [sk(11)], [sk(10)])
            P.act(lambda e: e.activation(out=S(ci_), in_=S(11), func=AF.Sin), reads=[sk(11)], writes=[sk(ci_)])
        cossin(12, 18, 19)
        cossin(15, 20, 21)
        cossin(16, 22, 23)
        cossin(17, 13, 14)
        P.act(lambda e: e.activation(out=SIN[:].rearrange("p s c -> p c s"), in_=ANG.rearrange("p (j s) -> p j s", s=16), func=AF.Sin),
              reads=XK, writes=["SIN"])
        P.dve(lambda e: e.tensor_scalar(out=TMPW, in0=ANG, scalar1=PI / 2, scalar2=None, op0=ALU.add), reads=XK, writes=XK)
        wrap(TMPW, s5flat[:, 0:CH * 16], XK, S5K)
        P.act(lambda e: e.activation(out=COS[:].rearrange("p s c -> p c s"), in_=TMPW.rearrange("p (j s) -> p j s", s=16), func=AF.Sin),
              reads=XK, writes=["COS"])

        ringF = ring[:].bitcast(F32)
        A = [ringF[:, 0:2048].rearrange("p (s c) -> p s c", s=16), ringF[:, 2048:4096].rearrange("p (s c) -> p s c", s=16)]
        AK = [[("w", 0), ("w", 1)], [("w", 2), ("w", 3)]]
        P.dma("sp", bnat[0][:], sb_re.rearrange("(s g) p h -> (g p) s h", g=2), writes=["bnat0"], key="bnat0")
        P.dma("sp", bnat[1][:], sb_im.rearrange("(s g) p h -> (g p) s h", g=2), writes=["bnat1"], key="bnat1")
        P.dma("sp", cnat[0][:], sc_re.rearrange("(m g) h p -> (g h) m p", g=8), writes=["cnat0"], key="cnat0")
        P.dma("sp", cnat[1][:], sc_im.rearrange("(m g) h p -> (g h) m p", g=8), writes=["cnat1"], key="cnat1")
        fre_b = S(8).unsqueeze(2).to_broadcast([128, 16, 16])
        fim_b = S(9).unsqueeze(2).to_broadcast([128, 16, 16])
        tt(btmp[0][:], bnat[0][:], fre_b, ALU.mult, ["bnat0", sk(8)], ["bt0"])
        tt(btmp[1][:], bnat[1][:], fim_b, ALU.mult, ["bnat1", sk(9)], ["bt1"])
        tt(btmp[2][:], bnat[1][:], fre_b, ALU.mult, ["bnat1", sk(8)], ["bt2"])
        tt(btmp[3][:], bnat[0][:], fim_b, ALU.mult, ["bnat0", sk(9)], ["bt3"])
        for i in range(2):
            P.dve(lambda e, i=i: e.memset(ringF[:, 2048 * i:2048 * (i + 1)], 0.0), writes=AK[i])
        tt(btmp[0][:], btmp[0][:], btmp[1][:], ALU.subtract, ["bt0", "bt1"], ["bt0"])
        tt(btmp[2][:], btmp[2][:], btmp[3][:], ALU.add, ["bt2", "bt3"], ["bt2"])

        def transposes(i, evac):
            for t in (3, 2, 1, 0):
                b = PS.alloc()
                for m_ in range(4):
                    s = 4 * m_ + t
                    P.pe(lambda e, s=s, m_=m_, b=b: e.transpose(out=ps[b][:, m_ * 128:(m_ + 1) * 128], in_=A[i][:, s, :], identity=idf[:]),
                         reads=AK[i] + ["idf"], writes=[psk(b)])
                evac(t, b)
                PS.free(b)

        for c in range(4):
            if c == 0:
                srcs = ((btmp[0], "bt0"), (btmp[2], "bt2"))
            else:
                a_b = S(16 + 2 * c).unsqueeze(2).to_broadcast([128, 16, 16])
                b_b = S(17 + 2 * c).unsqueeze(2).to_broadcast([128, 16, 16])
                ak, bk = sk(16 + 2 * c), sk(17 + 2 * c)
                tt(bnat[0][:], btmp[0][:], a_b, ALU.mult, ["bt0", ak, "bnat0"], ["bnat0"])
                tt(bnat[1][:], btmp[2][:], b_b, ALU.mult, ["bt2", bk, "bnat1"], ["bnat1"])
                tt(btmp[1][:], bnat[0][:], bnat[1][:], ALU.add, ["bnat0", "bnat1"], ["bt1"])
                tt(bnat[0][:], btmp[2][:], a_b, ALU.mult, ["bt2", ak, "bnat0"], ["bnat0"])
                tt(bnat[1][:], btmp[0][:], b_b, ALU.mult, ["bt0", bk, "bnat1"], ["bnat1"])
                tt(btmp[3][:], bnat[0][:], bnat[1][:], ALU.subtract, ["bnat0", "bnat1"], ["bt3"])
                srcs = ((btmp[1], "bt1"), (btmp[3], "bt3"))
            for i, (src, skey) in enumerate(srcs):
                for q_ in range(4):
                    for g2 in range(2):
                        c0 = q_ * 32 + g2 * 16
                        pr = slice(g2 * 64, (g2 + 1) * 64)
                        if i == 0:
                            P.dve(lambda e, q_=q_, pr=pr, c0=c0, src=src: e.tensor_copy(out=A[0][pr, q_:16:4, c0:c0 + 16], in_=src[pr, q_:16:4, :]), reads=[skey], writes=AK[0])
                        else:
                            P.act(lambda e, q_=q_, pr=pr, c0=c0, src=src: e.activation(out=A[1][pr, q_:16:4, c0:c0 + 16], in_=src[pr, q_:16:4, :], func=AF.Copy), reads=[skey], writes=AK[1])

                def evac_b(t, b, i=i, c=c):
                    lo = 64 if t == 3 else 32 * t
                    hi = 128 if t == 3 else 32 * t + 32
                    P.act(lambda e, b=b: e.activation(out=LBk[i][lo:hi, c, :, :], in_=ps[b][lo:hi, :].rearrange("p (m n) -> p m n", m=4), func=AF.Copy),
                          reads=[psk(b)], writes=["LBk%d" % i])
                transposes(i, evac_b)

        L0 = [xflat[:, 0:512].rearrange("p (s c) -> p s c", s=16), xflat[:, 512:1024].rearrange("p (s c) -> p s c", s=16)]
        LT = [xflat[:, 1024:1536].rearrange("p (s c) -> p s c", s=16), xflat[:, 1536:2048].rearrange("p (s c) -> p s c", s=16)]
        for i in range(2):
            for q_ in range(4):
                for g2 in range(2):
                    j = 2 * q_ + g2
                    msk = cmk[:, 2 + i, j:j + 1]
                    P.dve(lambda e, i=i, q_=q_, g2=g2, msk=msk: e.tensor_scalar(
                        out=A[i][:, q_:16:4, g2 * 64:(g2 + 1) * 64], in0=cnat[i][:, :, :], scalar1=msk, scalar2=None, op0=ALU.mult),
                        reads=["cnat%d" % i, "cmk2", "cmk3"], writes=AK[i])

            def evac_c(t, b, i=i):
                P.act(lambda e, b=b, t=t: e.activation(out=L0[i][:, t:16:4, :], in_=ps[b][:, :].rearrange("p (m n) -> p m n", m=4)[:, :, 32 * t:32 * t + 32], func=AF.Copy),
                      reads=[psk(b)], writes=XK)
            transposes(i, evac_c)
        P.dve(lambda e: e.tensor_copy(out=LCk[0][:, 0, :, :], in_=L0[0]), reads=XK, writes=["LCk0"])
        P.dve(lambda e: e.tensor_copy(out=LCk[1][:, 0, :, :], in_=L0[1]), reads=XK, writes=["LCk1"])
        for c in range(1, 4):
            a_b = S(16 + 2 * c).unsqueeze(2).to_broadcast([128, 16, 32])
            b_b = S(17 + 2 * c).unsqueeze(2).to_broadcast([128, 16, 32])
            ak, bk = sk(16 + 2 * c), sk(17 + 2 * c)
            tt(LT[0], L0[0], a_b, ALU.mult, XK + [ak], XK)
            tt(LT[1], L0[1], b_b, ALU.mult, XK + [bk], XK)
            tt(LCk[0][:, c, :, :], LT[0], LT[1], ALU.add, XK, ["LCk0"])
            tt(LT[0], L0[1], a_b, ALU.mult, XK + [ak], XK)
            tt(LT[1], L0[0], b_b, ALU.mult, XK + [bk], XK)
            tt(LCk[1][:, c, :, :], LT[0], LT[1], ALU.subtract, XK, ["LCk1"])
        for m in range(4):
            P.dve(lambda e, m=m: e.tensor_scalar(out=diagD[:, m, :], in0=idf[:], scalar1=dcol[:, m:m + 1], scalar2=None, op0=ALU.mult),
                  reads=["idf", "cols"], writes=["diagD"])
        P.dve(lambda e: e.memset(wlast[:], 0.0), writes=[("wlast", s_) for s_ in range(16)])
        P.dve(lambda e: e.memset(cvbuf, 0.0), writes=CVK)

        def rms_to_T(gcol, gkey, final=False, hook=None):
            for j in range(4):
                P.act(lambda e, j=j: e.activation(out=junk, in_=xres[:, j, :], func=AF.Square, accum_out=stat[:, j:j + 1]),
                      reads=[("x", j)], writes=["ost", ("stat", j)])
            SK = [("stat", j) for j in range(4)]
            P.dve(lambda e: e.tensor_scalar(out=stat[:, 4:8], in0=stat[:, 0:4], scalar1=1.0 / D, scalar2=1e-6, op0=ALU.mult, op1=ALU.add),
                  reads=SK, writes=["stat2"])
            P.act(lambda e: e.activation(out=stat[:, 4:8], in_=stat[:, 4:8], func=AF.Sqrt), reads=["stat2"], writes=["stat2"])
            P.dve(lambda e: e.reciprocal(out=stat[:, 8:12], in_=stat[:, 4:8]), reads=["stat2"], writes=["rstd"])
            if final:
                return
            for j in range(4):
                P.act(lambda e, j=j: e.activation(out=hn[:, j % 2, :], in_=xres[:, j, :], func=AF.Copy, scale=stat[:, 8 + j:9 + j]),
                      reads=[("x", j), "rstd"], writes=[("hn", j % 2)])
                if hook is not None:
                    hook(j)
                b = PS.alloc()
                pv = ps[b][:].bitcast(BF16)
                for kt in range(8):
                    P.pe(lambda e, j=j, kt=kt, pv=pv: e.transpose(out=pv[:, kt * 128:(kt + 1) * 128], in_=hn[:, j % 2, kt * 128:(kt + 1) * 128], identity=idb[:]),
                         reads=[("hn", j % 2), "idb"], writes=[psk(b)])
                P.dve(lambda e, j=j, pv=pv: e.tensor_tensor(
                    out=hT[:, :, j * 128:(j + 1) * 128], in0=pv.rearrange("p (k t) -> p k t", k=8),
                    in1=gcol.unsqueeze(2).to_broadcast([128, 8, 128]), op=ALU.mult),
                    reads=[psk(b), gkey], writes=[("hT", j)])
                PS.free(b)

        HTK = [("hT", j) for j in range(4)]

        def win_tile(ct, hchunk):
            WS.check(hchunk)
            _, wv, wk = hchunk
            b = PS.alloc()
            o = (ct % 2) * 128
            for kt in range(8):
                P.pe(lambda e, kt=kt, b=b, o=o, wv=wv: e.matmul(ps[b][:], lhsT=wv[:, kt, o:o + 128], rhs=hT[:, kt, :], start=(kt == 0), stop=(kt == 7)),
                     reads=[wk] + HTK, writes=[psk(b)])
            return b

        def mm4(hchunk, mo, rhs_t, rkeys):
            WS.check(hchunk)
            _, wv, wk = hchunk
            b = PS.alloc()
            o = (mo % 4) * 128
            for kt in range(4):
                P.pe(lambda e, kt=kt, b=b, o=o, wv=wv: e.matmul(ps[b][:], lhsT=wv[:, kt, o:o + 128], rhs=rhs_t[:, kt, :], start=(kt == 0), stop=(kt == 3)),
                     reads=[wk] + rkeys, writes=[psk(b)])
            return b

        def mixer_block(blk):
            tok0 = blk * TB
            first = (blk * TB) % SEQ == 0
            for j in range(4):
                P.dma("sp", xres[:, j, :], x_d[tok0 + j * 128:tok0 + (j + 1) * 128, :], writes=[("x", j)], key=("x", j))
            rms_to_T(gmix, "cols")
            for m in range(4):
                if m % 2 == 0:
                    hc = WS.get(("win", m // 2))
                b = win_tile(m, hc)
                P.act(lambda e, m=m, b=b: e.activation(out=uT[:, m, :], in_=ps[b][:], func=AF.Identity, bias=binc[:, m:m + 1], scale=1.0),
                      reads=[psk(b), "cols"], writes=[("uT", m)])
                PS.free(b)
            if first:
                P.dve(lambda e: e.memset(wlast[:], 0.0), writes=[("wlast", s_) for s_ in range(16)])
                P.dve(lambda e: e.memset(cvbuf[:, :, 0:2], 0.0), writes=CVK)
            cst = {}

            def conv_step(i):
                kind, m = divmod(i, 4)
                if kind == 0:
                    if m % 2 == 0:
                        cst["hc"] = WS.get(("win", 4 + m // 2))
                    b = win_tile(8 + m, cst["hc"])
                    P.act(lambda e, m=m, b=b: e.activation(out=abuf[:, m, :], in_=ps[b][:], func=AF.Identity, bias=binc[:, 8 + m:9 + m], scale=1.0),
                          reads=[psk(b), "cols"], writes=[("abuf", m)])
                    PS.free(b)
                elif kind == 1:
                    if m % 2 == 0:
                        cst["hc"] = WS.get(("win", 6 + m // 2))
                    b = win_tile(12 + m, cst["hc"])
                    P.dve(lambda e, m=m, b=b: e.scalar_tensor_tensor(out=cvbuf[:, m, 2:TB + 2], in0=ps[b][:], scalar=binc[:, 12 + m:13 + m], in1=abuf[:, m, :],
                                                                     op0=ALU.add, op1=ALU.mult), reads=[psk(b), "cols", ("abuf", m)], writes=[("cv", m)])
                    PS.free(b)
                    P.dve(lambda e, m=m: e.tensor_scalar(out=abuf[:, m, :], in0=cvbuf[:, m, 2:TB + 2], scalar1=cw[:, 8 + m:9 + m], scalar2=cbc[:, m:m + 1],
                                                         op0=ALU.mult, op1=ALU.add), reads=[("cv", m), "cols"], writes=[("abuf", m)])
                    P.dve(lambda e, m=m: e.scalar_tensor_tensor(out=abuf[:, m, :], in0=cvbuf[:, m, 1:TB + 1], scalar=cw[:, 4 + m:5 + m], in1=abuf[:, m, :],
                                                                op0=ALU.mult, op1=ALU.add), reads=[("cv", m), "cols", ("abuf", m)], writes=[("abuf", m)])
                    P.dve(lambda e, m=m: e.scalar_tensor_tensor(out=abuf[:, m, :], in0=cvbuf[:, m, 0:TB], scalar=cw[:, m:m + 1], in1=abuf[:, m, :],
                                                                op0=ALU.mult, op1=ALU.add), reads=[("cv", m), "cols", ("abuf", m)], writes=[("abuf", m)])
                    P.act(lambda e, m=m: e.activation(out=cvbuf[:, m, 0:2], in_=cvbuf[:, m, TB:TB + 2], func=AF.Copy), reads=[("cv", m), ("abuf", m)], writes=[("cv", m)])
                else:
                    if m % 2 == 0:
                        cst["hc"] = WS.get(("win", 2 + m // 2))
                    b = win_tile(4 + m, cst["hc"])
                    P.dve(lambda e, m=m, b=b: e.scalar_tensor_tensor(out=gbT[:, m, :], in0=ps[b][:], scalar=binc[:, 4 + m:5 + m], in1=abuf[:, m, :],
                                                                     op0=ALU.add, op1=ALU.mult), reads=[psk(b), "cols", ("abuf", m)], writes=[("gbT", m)])
                    PS.free(b)

            yb = None
            SETS = ((s5t, "s5t"), (s5u, "s5u"))
            for pr in range(8):
                if 1 <= pr <= 6:
                    conv_step(2 * (pr - 1))
                    conv_step(2 * (pr - 1) + 1)
                m = pr // 2
                tiles = (2 * pr, 2 * pr + 1)
                banks = []
                for t, s in enumerate(tiles):
                    bxr = PS.alloc()
                    bxi = PS.alloc()
                    q = s % 4
                    kw = {"tile_position": (96, 0)} if q == 3 else {}
                    for c in range(4):
                        for i, bb in ((0, bxr), (1, bxi)):
                            P.pe(lambda e, q=q, m=m, c=c, i=i, b=bb, kw=kw: e.matmul(ps[b][:, c * 128:(c + 1) * 128], lhsT=LBk[i][32 * q:32 * q + 32, c, m, :],
                                                                                    rhs=uT[32 * q:32 * q + 32, m, c * 128:(c + 1) * 128], start=True, stop=True, **kw),
                                 reads=["LBk%d" % i, ("uT", m)], writes=[psk(bb)])
                    banks.append((bxr, bxi))

                def v3(ap):
                    return ap.rearrange("p (a c) -> p a c", a=4)
                ctx = []
                for t, s in enumerate(tiles):
                    buf, nm = SETS[t]
                    T = [buf[:, i, :] for i in range(4)]
                    TK = [(nm, i) for i in range(4)]
                    cosb = COS[:, s, :].unsqueeze(1).to_broadcast([128, 4, CH])
                    sinb = SIN[:, s, :].unsqueeze(1).to_broadcast([128, 4, CH])
                    ctx.append((s, T, TK, cosb, sinb, banks[t]))
                for step in range(6):
                    for (s, T, TK, cosb, sinb, (bxr, bxi)) in (ctx if step in (0, 1) else ctx[::-1]):
                        xr, xi = ps[bxr][:], ps[bxi][:]
                        if step == 0:
                            tt(v3(T[0]), v3(xr), cosb, ALU.mult, [psk(bxr), "COS"], [TK[0]])
                        elif step == 2:
                            tt(v3(T[1]), v3(xi), sinb, ALU.mult, [psk(bxi), "SIN"], [TK[1]])
                        elif step == 1:
                            tt(v3(T[2]), v3(xi), cosb, ALU.mult, [psk(bxi), "COS"], [TK[2]])
                        elif step == 3:
                            tt(v3(T[3]), v3(xr), sinb, ALU.mult, [psk(bxr), "SIN"], [TK[3]])
                        elif step == 4:
                            tt(T[0], T[0], T[1], ALU.add, [TK[0], TK[1]], [TK[0]])
                        else:
                            tt(T[2], T[2], T[3], ALU.subtract, [TK[2], TK[3]], [TK[2]])
                for (bxr, bxi) in banks:
                    PS.free(bxr)
                    PS.free(bxi)
                for step in range(4):
                    for t, (s, T, TK, cosb, sinb, _) in enumerate(ctx):
                        cE, sE = S(13)[:, s:s + 1], S(14)[:, s:s + 1]
                        rb = S(1)[:, s:s + 1].to_broadcast([128, TB])
                        lr, li = wlast[:, s, 0:1], wlast[:, s, 1:2]
                        lk = [("wlast", s)]
                        ck = [("chn", t, i) for i in range(4)]
                        if step == 0:
                            P.dve(lambda e, li=li, sE=sE, t=t: e.tensor_scalar(out=chn[:, t, 0:1], in0=li, scalar1=sE, scalar2=None, op0=ALU.mult), reads=lk + [sk(14)], writes=[ck[0]])
                            P.dve(lambda e, li=li, cE=cE, t=t: e.tensor_scalar(out=chn[:, t, 2:3], in0=li, scalar1=cE, scalar2=None, op0=ALU.mult), reads=lk + [sk(13)], writes=[ck[2]])
                        elif step == 1:
                            P.dve(lambda e, lr=lr, cE=cE, t=t: e.scalar_tensor_tensor(out=chn[:, t, 1:2], in0=lr, scalar=cE, in1=chn[:, t, 0:1], op0=ALU.mult, op1=ALU.subtract),
                                  reads=lk + [sk(13), ck[0]], writes=[ck[1]])
                            P.dve(lambda e, lr=lr, sE=sE, t=t: e.scalar_tensor_tensor(out=chn[:, t, 3:4], in0=lr, scalar=sE, in1=chn[:, t, 2:3], op0=ALU.mult, op1=ALU.add),
                                  reads=lk + [sk(14), ck[2]], writes=[ck[3]])
                        elif step == 2:
                            P.dve(lambda e, rb=rb, T=T, t=t: e.tensor_tensor_scan(out=T[1], data0=rb, data1=T[0], initial=chn[:, t, 1:2], op0=ALU.mult, op1=ALU.add),
                                  reads=[TK[0], ck[1], sk(1)], writes=[TK[1]])
                        else:
                            P.dve(lambda e, rb=rb, T=T, t=t: e.tensor_tensor_scan(out=T[3], data0=rb, data1=T[2], initial=chn[:, t, 3:4], op0=ALU.mult, op1=ALU.add),
                                  reads=[TK[2], ck[3], sk(1)], writes=[TK[3]])
                for t, (s, T, TK, cosb, sinb, _) in enumerate(ctx):
                    P.act(lambda e, s=s, T=T: e.activation(out=wlast[:, s, 0:1], in_=T[1][:, TB - 1:TB], func=AF.Copy), reads=[TK[1]], writes=[("wlast", s)])
                    P.act(lambda e, s=s, T=T: e.activation(out=wlast[:, s, 1:2], in_=T[3][:, TB - 1:TB], func=AF.Copy), reads=[TK[3]], writes=[("wlast", s)])
                for t, (s, T, TK, cosb, sinb, _) in enumerate(ctx):
                    sr_o, si_o = sri[:, t, 0, :], sri[:, t, 1, :]
                    SRK = [("sri", t, 0), ("sri", t, 1)]
                    Q0, Q1 = s5p[:, 0, :], s5p[:, 1, :]
                    QK = [("s5p", 0), ("s5p", 1)]

                    if t == 1:
                        Q0, Q1 = T[0], T[2]
                        QK = [TK[0], TK[2]]

                    def pt(out, a, b, op, reads, writes, t=t):
                        if t == 0:
                            P.pool(lambda e: e.tensor_tensor(out=out, in0=a, in1=b, op=op), reads=reads, writes=writes)
                        else:
                            P.dve(lambda e: e.tensor_tensor(out=out, in0=a, in1=b, op=op), reads=reads, writes=writes)
                    pt(v3(Q0), v3(T[1]), cosb, ALU.mult, [TK[1], "COS"], [QK[0]])
                    pt(v3(Q1), v3(T[3]), sinb, ALU.mult, [TK[3], "SIN"], [QK[1]])
                    pt(sr_o, Q0, Q1, ALU.subtract, QK, [SRK[0]])
                    pt(v3(Q0), v3(T[1]), sinb, ALU.mult, [TK[1], "SIN"], [QK[0]])
                    pt(v3(Q1), v3(T[3]), cosb, ALU.mult, [TK[3], "COS"], [QK[1]])
                    pt(si_o, Q0, Q1, ALU.add, QK, [SRK[1]])
                    q = s % 4
                    if q == 0:
                        yb = PS.alloc()
                        P.pe(lambda e, m=m, b=yb: e.matmul(ps[b][:], lhsT=diagD[:, m, :], rhs=uT[:, m, :], start=True, stop=False),
                             reads=["diagD", ("uT", m)], writes=[psk(yb)])
                    kw = {"tile_position": (0, 96)} if q == 3 else {}
                    for c in range(4):
                        for i, (src, skey) in enumerate(((sr_o, SRK[0]), (si_o, SRK[1]))):
                            last = (q == 3 and c == 3 and i == 1)
                            P.pe(lambda e, s=s, q=q, c=c, i=i, b=yb, src=src, kw=kw, last=last: e.matmul(
                                ps[b][32 * q:32 * q + 32, c * 128:(c + 1) * 128], lhsT=LCk[i][:, c, s, :], rhs=src[:, c * 128:(c + 1) * 128], start=False, stop=last, **kw),
                                reads=["LCk%d" % i, skey], writes=[psk(yb)])
                    if s % 4 == 3:
                        P.act(lambda e, m=m, b=yb: e.activation(out=ybf[:, m, :], in_=ps[b][:], func=AF.Gelu_apprx_tanh), reads=[psk(yb)], writes=[("ybf", m)])
                        PS.free(yb)
            emit_prepass(12)
            YBK = [("ybf", m) for m in range(4)]
            GBK = [("gbT", m) for m in range(4)]
            TA, TBb, TC = s5t[:, 0, :], s5t[:, 1, :], s5t[:, 2, :]
            for mo in range(8):
                if mo % 4 == 0:
                    ha = WS.get(("glua", mo // 4))
                    hb = WS.get(("glub", mo // 4))
                if mo % 2 == 0:
                    hg = WS.get(("win", 8 + mo // 2))
                ba = mm4(ha, mo, ybf, YBK)
                bb = mm4(hb, mo, ybf, YBK)
                bg = win_tile(16 + mo, hg)
                P.act(lambda e, b=bb: e.activation(out=TA, in_=ps[b][:], func=AF.Sigmoid), reads=[psk(bb)], writes=[("s5t", 0)])
                P.act(lambda e, b=bg, mo=mo: e.activation(out=TBb, in_=ps[b][:], func=AF.Sigmoid, bias=binc[:, 16 + mo:17 + mo], scale=1.0),
                      reads=[psk(bg), "cols"], writes=[("s5t", 1)])
                tt(TA, ps[ba][:], TA, ALU.mult, [psk(ba), ("s5t", 0)], [("s5t", 0)])
                tt(mixT[:, mo, :], TA, TBb, ALU.mult, [("s5t", 0), ("s5t", 1)], [("mixT", mo)])
                PS.free(ba)
                PS.free(bb)
                PS.free(bg)
            for mo in range(8):
                if mo % 4 == 0:
                    hco = WS.get(("wco", mo // 4))
                if mo % 2 == 0:
                    hg = WS.get(("win", 12 + mo // 2))
                by = mm4(hco, mo, gbT, GBK)
                bg = win_tile(24 + mo, hg)
                P.act(lambda e, b=bg, mo=mo: e.activation(out=TC, in_=ps[b][:], func=AF.Sigmoid, bias=binc[:, 24 + mo:25 + mo], scale=1.0),
                      reads=[psk(bg), "cols"], writes=[("s5t", 2)])
                tt(TC, ps[by][:], TC, ALU.mult, [psk(by), ("s5t", 2)], [("s5t", 2)])
                tt(mixT[:, mo, :], mixT[:, mo, :], TC, ALU.add, [("mixT", mo), ("s5t", 2)], [("mixT", mo)])
                PS.free(by)
                PS.free(bg)
            MXK = [("mixT", mo) for mo in range(8)]
            for hf in range(2):
                h0 = WS.get(("wo", hf, 0))
                h1 = WS.get(("wo", hf, 1))
                for j in range(4):
                    b = PS.alloc()
                    for kt in range(8):
                        hcur = h0 if kt < 4 else h1
                        WS.check(hcur)
                        P.pe(lambda e, j=j, kt=kt, b=b, wv=hcur[1]: e.matmul(ps[b][:], lhsT=mixT[:, kt, j * 128:(j + 1) * 128], rhs=wv[:, kt % 4, :],
                                                                          start=(kt == 0), stop=(kt == 7)), reads=[hcur[2]] + MXK, writes=[psk(b)])
                    xs = xres[:, j, hf * 512:(hf + 1) * 512]
                    tt(xs, xs, ps[b][:], ALU.add, [("x", j), psk(b)], [("x", j)])
                    PS.free(b)

        NU = 96
        gffnB = P.sb("gffnB", [128, D], F32)
        ustr = P.sb("ustr", [128, 128], BF16)
        onesb = P.sb("onesb", [128, 128], BF16)
        m12b = P.sb("m12b", [128, 4, 32], BF16)
        rq = P.sb("rq", [128, 8, 16], F32)
        rw = P.sb("rw", [128, 5, 128], F32)
        m1s = P.sb("m1s", [128, 32, 32], BF16)
        m2s = P.sb("m2s", [128, 32, 32], BF16)
        srt = P.sb("srt", [128, 12, 32], F32)
        posi = P.sb("posi", [128, 2, 32], I32)
        uv = P.sb("uv", [128, 3, NU], F32)
        idxw = P.sb("idxw", [128, NU], I32)
        pidx = P.sb("pidx", [128, 1], F32)
        st3 = P.sb("st3", [128, 32], F32)
        junk3b = P.sb("junk3b", [128, D], BF16)
        h2b = P.sb("h2b", [128, 2, D], BF16)
        sgu = P.sb("sgu", [128, 2, 256], F32)
        hidU = P.sb("hidU", [128, 2, 2, 128], BF16)
        P.dma("sp", gffnB[:], norm_ffn.partition_broadcast(128), writes=["gffnB"], key="gffnB")
        P.dve(lambda e: e.tensor_single_scalar(out=ustr[:], in_=idi[:], scalar=0.0, op=ALU.is_gt), reads=["idi"], writes=["ustr"])
        P.dve(lambda e: e.memset(onesb[:], 1.0), writes=["onesb"])
        P.dve(lambda e: e.memset(srt[:], 0.0), writes=["srt"])
        P.dve(lambda e: e.memset(srt[:, 9, :], 1.0), reads=["srt"], writes=["srt9"])
        P.pool(lambda e: e.iota(srt[:, 5, :], pattern=[[128, 32]], base=0, channel_multiplier=0, allow_small_or_imprecise_dtypes=True), reads=["srt"], writes=["srt5"])
        P.pool(lambda e: e.iota(uv[:, 0, :], pattern=[[1, NU]], base=0, channel_multiplier=0, allow_small_or_imprecise_dtypes=True), writes=["uv0"])
        P.pool(lambda e: e.iota(pidx[:], pattern=[[0, 1]], base=0, channel_multiplier=1, allow_small_or_imprecise_dtypes=True), writes=["pidx"])
        P.dve(lambda e: e.memset(m1s[:], 0.0), writes=["m1s"])
        P.dve(lambda e: e.memset(m2s[:], 0.0), writes=["m2s"])
        carry = srt[:, 0, :]
        HSW = []
        YSK = [("ys", u) for u in range(NU)]

        def h2_hook(j, blk):
            J = blk * 4 + j
            P.dve(lambda e, j=j: e.tensor_tensor(out=h2b[:, j % 2, :], in0=hn[:, j % 2, :], in1=gffnB[:], op=ALU.mult),
                  reads=[("hn", j % 2), "gffnB"], writes=[("h2b", j % 2)])
            P.dma("sp", h2_s[J * 128:(J + 1) * 128, :], h2b[:, j % 2, :], reads=[("h2b", j % 2)], writes=[("h2s", J)], key=("h2b", j % 2))

        def route_block(blk):
            tok0 = blk * TB
            J0 = blk * 4
            for j in range(4):
                P.dma("sp", x1_s[tok0 + j * 128:tok0 + (j + 1) * 128, :], xres[:, j, :], reads=[("x", j)], writes=[("x1s", blk, j)], key=("x1st", j))
            rms_to_T(gffn, "cols", hook=lambda j: h2_hook(j, blk))
            b = PS.alloc()
            for j in range(4):
                for kt in range(8):
                    P.pe(lambda e, j=j, kt=kt, b=b: e.matmul(ps[b][:, j * 36:(j + 1) * 36], lhsT=hT[:, kt, j * 128:(j + 1) * 128], rhs=wr[:, kt, :], start=(kt == 0), stop=(kt == 7)),
                         reads=HTK + WRK, writes=[psk(b)])
            P.dve(lambda e, b=b: e.tensor_tensor(out=lsb[:], in0=ps[b][:, 0:144].rearrange("p (j n) -> p j n", j=4), in1=brB[:].unsqueeze(1).to_broadcast([128, 4, 36]), op=ALU.add),
                  reads=[psk(b)] + BRB, writes=["lsb"])
            PS.free(b)
            lg = lsb[:, :, 0:4]
            le4 = lsb[:, :, 4:36].rearrange("p j (g k) -> p j g k", g=4)
            q4 = lambda i: rq[:, i, 0:4]
            q16 = lambda i: rq[:, i, 0:16].rearrange("p (j g) -> p j g", j=4)
            W3 = lambda i: rw[:, i, :].rearrange("p (j n) -> p j n", j=4)
            bc4 = lambda ap: ap.unsqueeze(2).to_broadcast([128, 4, 4])
            bc32 = lambda ap: ap.unsqueeze(2).to_broadcast([128, 4, 32])
            P.dve(lambda e: e.tensor_reduce(out=q4(0), in_=lg, axis=AX.X, op=ALU.max), reads=["lsb"], writes=["q0"])
            P.dve(lambda e: e.tensor_tensor(out=q16(1), in0=lg, in1=bc4(q4(0)), op=ALU.is_ge), reads=["lsb", "q0"], writes=["q1"])
            P.dve(lambda e: e.tensor_scalar(out=rq[:, 2, 0:16], in0=rq[:, 1, 0:16], scalar1=BIG, scalar2=-BIG, op0=ALU.mult, op1=ALU.add), reads=["q1"], writes=["q2"])
            P.dve(lambda e: e.tensor_tensor(out=q16(3), in0=lg, in1=bc4(q4(0)), op=ALU.subtract), reads=["lsb", "q0"], writes=["q3"])
            P.act(lambda e: e.activation(out=rq[:, 3, 0:16], in_=rq[:, 3, 0:16], func=AF.Exp), reads=["q3"], writes=["q3"])
            P.dve(lambda e: e.tensor_reduce(out=q4(4), in_=q16(3), axis=AX.X, op=ALU.add), reads=["q3"], writes=["q4"])
            P.dve(lambda e: e.reciprocal(out=q4(4), in_=q4(4)), reads=["q4"], writes=["q4"])
            P.dve(lambda e: e.tensor_tensor(out=rw[:, 0, :].rearrange("p (j g k) -> p j g k", j=4, g=4), in0=le4,
                                            in1=q16(2).unsqueeze(3).to_broadcast([128, 4, 4, 8]), op=ALU.add), reads=["lsb", "q2"], writes=["w0"])
            P.dve(lambda e: e.tensor_reduce(out=q4(5), in_=W3(0), axis=AX.X, op=ALU.max), reads=["w0"], writes=["q5"])
            P.dve(lambda e: e.tensor_tensor(out=W3(1), in0=W3(0), in1=bc32(q4(5)), op=ALU.is_ge), reads=["w0", "q5"], writes=["w1"])
            P.dve(lambda e: e.scalar_tensor_tensor(out=W3(2), in0=W3(1), scalar=-BIG, in1=W3(0), op0=ALU.mult, op1=ALU.add), reads=["w1", "w0"], writes=["w2"])
            P.dve(lambda e: e.tensor_reduce(out=q4(6), in_=W3(2), axis=AX.X, op=ALU.max), reads=["w2"], writes=["q6"])
            P.dve(lambda e: e.tensor_tensor(out=W3(3), in0=W3(0), in1=bc32(q4(6)), op=ALU.is_ge), reads=["w0", "q6"], writes=["w3"])
            P.dve(lambda e: e.tensor_copy(out=m12b[:], in_=W3(3)), reads=["w3"], writes=["m12b"])
            P.dve(lambda e: e.tensor_copy(out=m1s[:, J0:J0 + 4, :], in_=W3(1)), reads=["w1", "m1s"], writes=[("m1s", J0)])
            P.dve(lambda e: e.tensor_tensor(out=m2s[:, J0:J0 + 4, :], in0=W3(3), in1=W3(1), op=ALU.subtract), reads=["w3", "w1", "m2s"], writes=[("m2s", J0)])
            P.dve(lambda e: e.tensor_tensor(out=q4(7), in0=q4(5), in1=q4(6), op=ALU.add), reads=["q5", "q6"], writes=["q7"])
            P.dve(lambda e: e.tensor_scalar(out=rw[:, 2, :], in0=rw[:, 0, :], scalar1=2.0, scalar2=None, op0=ALU.mult), reads=["w0", "w2"], writes=["w2"])
            P.dve(lambda e: e.tensor_tensor(out=W3(2), in0=W3(2), in1=bc32(q4(7)), op=ALU.subtract), reads=["w2", "q7"], writes=["w2"])
            P.act(lambda e: e.activation(out=rw[:, 2, :], in_=rw[:, 2, :], func=AF.Sigmoid), reads=["w2"], writes=["w2"])
            P.dve(lambda e: e.tensor_tensor(out=W3(2), in0=W3(2), in1=bc32(q4(4)), op=ALU.mult), reads=["w2", "q4"], writes=["w2"])
            P.dve(lambda e: e.tensor_tensor(out=W3(2), in0=W3(2), in1=W3(3), op=ALU.mult), reads=["w2", "w3"], writes=["w2"])
            b = PS.alloc()
            for j in range(4):
                P.pe(lambda e, b=b, j=j: e.matmul(ps[b][:, j * 64:j * 64 + 32], lhsT=ustr[:], rhs=m12b[:, j, :], start=True, stop=True), reads=["ustr", "m12b"], writes=[psk(b)])
                P.pe(lambda e, b=b, j=j: e.matmul(ps[b][:, j * 64 + 32:j * 64 + 64], lhsT=onesb[:], rhs=m12b[:, j, :], start=True, stop=True), reads=["onesb", "m12b"], writes=[psk(b)])
            for j in range(4):
                tt(rw[:, 4, j * 32:(j + 1) * 32], ps[b][:, j * 64:j * 64 + 32], carry, ALU.add, [psk(b), "srt", "carry"], [("w4", j)])
                tt(carry, carry, ps[b][:, j * 64 + 32:j * 64 + 64], ALU.add, [psk(b), "srt", "carry", ("w4", j)], ["carry"])
            PS.free(b)
            W4K = [("w4", j) for j in range(4)]
            P.dve(lambda e: e.tensor_tensor(out=W3(0), in0=W3(3), in1=W3(1), op=ALU.subtract), reads=["w3", "w1", "w2"], writes=["w0"])
            for (msk, mk, col_r, col_w) in ((W3(1), "w1", 1, 3), (W3(0), "w0", 2, 4)):
                P.dve(lambda e, msk=msk: e.tensor_tensor(out=W3(3), in0=msk, in1=W3(4), op=ALU.mult), reads=[mk, "w0"] + W4K, writes=["w3"])
                P.dve(lambda e, col_r=col_r: e.tensor_reduce(out=srt[:, col_r, J0:J0 + 4], in_=W3(3), axis=AX.X, op=ALU.add), reads=["w3", "srt"], writes=[("srtc", col_r, J0)])
                P.dve(lambda e, msk=msk: e.tensor_tensor(out=W3(3), in0=msk, in1=W3(2), op=ALU.mult), reads=[mk, "w2"], writes=["w3"])
                P.dve(lambda e, col_w=col_w: e.tensor_reduce(out=srt[:, col_w, J0:J0 + 4], in_=W3(3), axis=AX.X, op=ALU.add), reads=["w3", "srt"], writes=[("srtc", col_w, J0)])

        NJ = nblk * 4
        SRTC = [("srtc", c, 4 * b_) for c in (1, 2, 3, 4) for b_ in range(nblk)]

        def sort_phase():
            cmp3 = xflat[:, 0:1024].rearrange("p (a b) -> p a b", a=32)
            tiles, endc, base, ones32 = srt[:, 6, :], srt[:, 7, :], srt[:, 8, :], srt[:, 9, :]
            P.dve(lambda e: e.tensor_tensor(out=cmp3, in0=carry.unsqueeze(2).to_broadcast([128, 32, 32]), in1=srt[:, 5, :].unsqueeze(1).to_broadcast([128, 32, 32]), op=ALU.is_gt),
                  reads=["carry", "srt5"], writes=XK)
            P.dve(lambda e: e.tensor_reduce(out=tiles, in_=cmp3, axis=AX.X, op=ALU.add), reads=XK + ["srt"], writes=["srt6"])
            P.dve(lambda e: e.tensor_tensor_scan(out=endc, data0=ones32, data1=tiles, initial=0.0, op0=ALU.mult, op1=ALU.add), reads=["srt6", "srt9"], writes=["srt7"])
            tt(base, endc, tiles, ALU.subtract, ["srt6", "srt7"], ["srt8"])
            P.dve(lambda e: e.tensor_scalar(out=base, in0=base, scalar1=128.0, scalar2=None, op0=ALU.mult), reads=["srt8"], writes=["srt8"])
            for k, ms, mall in ((0, m1s, "m1s"), (1, m2s, "m2s")):
                MK = [(mall, 4 * b_) for b_ in range(nblk)] + [mall]
                P.dve(lambda e, ms=ms: e.tensor_tensor(out=cmp3, in0=ms[:], in1=base.unsqueeze(1).to_broadcast([128, 32, 32]), op=ALU.mult), reads=MK + ["srt8"], writes=XK)
                P.dve(lambda e, k=k: e.tensor_reduce(out=srt[:, 10 + k, :], in_=cmp3, axis=AX.X, op=ALU.add), reads=XK, writes=[("srt1x", k)])
                P.dve(lambda e, k=k: e.tensor_tensor(out=srt[:, 10 + k, :], in0=srt[:, 10 + k, :], in1=srt[:, 1 + k, :], op=ALU.add), reads=[("srt1x", k)] + SRTC, writes=[("srt1x", k)])
                P.dve(lambda e, k=k: e.tensor_copy(out=posi[:, k, :], in_=srt[:, 10 + k, :]), reads=[("srt1x", k)], writes=[("posi", k)])
            cmpu = xflat[:, 0:NU * 32].rearrange("p (a b) -> p a b", a=NU)
            P.dve(lambda e: e.tensor_tensor(out=cmpu, in0=uv[:, 0, :].unsqueeze(2).to_broadcast([128, NU, 32]), in1=endc.unsqueeze(1).to_broadcast([128, NU, 32]), op=ALU.is_ge),
                  reads=["uv0", "srt7"], writes=XK)
            P.dve(lambda e: e.tensor_reduce(out=uv[:, 1, :], in_=cmpu, axis=AX.X, op=ALU.add), reads=XK, writes=["uv1"])
            P.dve(lambda e: e.tensor_scalar(out=uv[:, 1, :], in0=uv[:, 1, :], scalar1=31.0, scalar2=None, op0=ALU.min), reads=["uv1"], writes=["uv1"])
            P.dve(lambda e: e.tensor_scalar(out=uv[:, 2, :], in0=uv[:, 1, :], scalar1=128.0, scalar2=pidx[:, 0:1], op0=ALU.mult, op1=ALU.add), reads=["uv1", "pidx"], writes=["uv2"])
            P.dve(lambda e: e.tensor_copy(out=idxw[:], in_=uv[:, 2, :]), reads=["uv2"], writes=["idxw"])

        def scatter_phase():
            hsb = mixT[:].rearrange("p a b -> p (a b)")[:, 0:2048].rearrange("p (r c) -> p r c", r=2)
            for J in range(NJ):
                par = J % 2
                hk = [("mixT", 2 * par), ("mixT", 2 * par + 1)]
                P.dma("sp", hsb[:, par, :], h2_s[J * 128:(J + 1) * 128, :], reads=[("h2s", J)], writes=hk, key=("hsb", par))
                for k in range(2):
                    key = ("hsw", J, k)
                    HSW.append(key)
                    P.add("pool", lambda e, J=J, k=k, par=par: e.indirect_dma_start(
                        out=hs, out_offset=bass.IndirectOffsetOnAxis(ap=posi[:, k, J:J + 1], axis=0), in_=hsb[:, par, :], in_offset=None),
                        reads=hk + [("posi", k)], writes=[key], dma_key=("hsc", par, k))

        arenaF = arena[:]
        s5f = s5t[:].rearrange("p a b -> p (a b)")
        def flatb(t):
            return t[:].rearrange("p a b -> p (a b)")
        WBUF = [(arenaF[:, 0:1024].bitcast(BF16), [("abuf", 0), ("abuf", 1)]), (arenaF[:, 1024:2048].bitcast(BF16), [("abuf", 2), ("abuf", 3)]),
                (arenaF[:, 2048:3072].bitcast(BF16), [("cv", 0), ("cv", 1)]), (arenaF[:, 3072:4096].bitcast(BF16), [("cv", 1), ("cv", 2), ("cv", 3)]),
                (s5f[:, 0:1024].bitcast(BF16), [("s5t", 0), ("s5t", 1)]), (s5f[:, 1024:2048].bitcast(BF16), [("s5t", 2), ("s5t", 3)]),
                (flatb(ybf), [("ybf", m) for m in range(4)]), (flatb(gbT), [("gbT", m) for m in range(4)]), (flatb(uT), [("uT", m) for m in range(4)])]

        def unit_phase():
            hsbT = mixT[:].rearrange("p a b -> p (a b)")[:, 2048:4096].rearrange("p (r k c) -> p r k c", r=2, k=8)
            hsl = mixT[:].rearrange("p a b -> p (a b)")[:, 0:2048].rearrange("p (r c) -> p r c", r=2)
            yo = xres[:].rearrange("p a b -> p (a b)").rearrange("p (r c) -> p r c", r=4)
            state = {}

            wsets = {}

            def gathers(u):
                st = u % 3
                wv = []
                for i, wsrc in enumerate((wgp, wup, wdp)):
                    ap, keys = WBUF[3 * st + i]
                    P.add("pool", lambda e, ap=ap, wsrc=wsrc, u=u: e.indirect_dma_start(
                        out=ap, out_offset=None, in_=wsrc, in_offset=bass.IndirectOffsetOnAxis(ap=idxw[:, u:u + 1], axis=0)),
                        reads=["idxw"] + WPK[i], writes=keys, dma_key=("wbuf", st, i))
                    wv.append((ap, keys))
                wsets[u] = wv

            def front(u):
                par = u % 2
                hk = [("mixT", 2 * par), ("mixT", 2 * par + 1)]
                tk = [("mixT", 4 + 2 * par), ("mixT", 5 + 2 * par)]
                if u == 0:
                    P.dma("sp", hsl[:, 0, :], hs[0:128, :], reads=HSW, writes=hk, key=("hsl", 0))
                if u + 1 < NU:
                    p1 = (u + 1) % 2
                    P.dma("sp", hsl[:, p1, :], hs[(u + 1) * 128:(u + 2) * 128, :], reads=HSW, writes=[("mixT", 2 * p1), ("mixT", 2 * p1 + 1)], key=("hsl", p1))
                wv = wsets.pop(u)
                b = PS.alloc()
                pv = ps[b][:].bitcast(BF16)
                for kt in range(8):
                    P.pe(lambda e, kt=kt, pv=pv, par=par: e.transpose(out=pv[:, kt * 128:(kt + 1) * 128], in_=hsl[:, par, kt * 128:(kt + 1) * 128], identity=idb[:]),
                         reads=hk + ["idb"], writes=[psk(b)])
                P.act(lambda e, pv=pv, par=par: e.activation(out=hsbT[:, par, :, :].rearrange("p k c -> p (k c)"), in_=pv, func=AF.Copy), reads=[psk(b)], writes=tk)
                PS.free(b)
                b = PS.alloc()
                for gi, (ap, keys) in enumerate(wv[0:2]):
                    w3 = ap.rearrange("p (k c) -> p k c", k=8)
                    for ht in range(2):
                        o = gi * 256 + ht * 128
                        for kt in range(8):
                            P.pe(lambda e, kt=kt, b=b, o=o, w3=w3, ht=ht, par=par: e.matmul(ps[b][:, o:o + 128], lhsT=w3[:, kt, ht * 128:(ht + 1) * 128], rhs=hsbT[:, par, kt, :],
                                                                                         start=(kt == 0), stop=(kt == 7)), reads=keys + tk, writes=[psk(b)])
                P.act(lambda e, b=b, par=par: e.activation(out=sgu[:, par, :], in_=ps[b][:, 0:256], func=AF.Silu), reads=[psk(b)], writes=[("sgu", par)])
                P.dve(lambda e, b=b, par=par: e.tensor_tensor(out=hidU[:, par, :, :].rearrange("p a b -> p (a b)"), in0=ps[b][:, 256:512], in1=sgu[:, par, :], op=ALU.mult),
                      reads=[psk(b), ("sgu", par)], writes=[("hidU", par)])
                PS.free(b)
                state[u] = wv[2]

            def back(u):
                par = u % 2
                q = u % 4
                ap, keys = state.pop(u)
                w3 = ap.rearrange("p (k c) -> p k c", k=2)
                yk = [("x", q)]
                for hf in range(2):
                    b = PS.alloc()
                    for ht in range(2):
                        P.pe(lambda e, hf=hf, ht=ht, b=b, w3=w3, par=par: e.matmul(ps[b][:], lhsT=hidU[:, par, ht, :], rhs=w3[:, ht, hf * 512:(hf + 1) * 512], start=(ht == 0), stop=(ht == 1)),
                             reads=keys + [("hidU", par)], writes=[psk(b)])
                    dst = xres[:, q, hf * 512:(hf + 1) * 512]
                    if hf == 0:
                        P.act(lambda e, b=b, dst=dst: e.activation(out=dst, in_=ps[b][:], func=AF.Copy), reads=[psk(b)], writes=yk)
                    else:
                        P.dve(lambda e, b=b, dst=dst: e.tensor_copy(out=dst, in_=ps[b][:]), reads=[psk(b)], writes=yk)
                    PS.free(b)
                P.dma("sp", ys[u * 128:(u + 1) * 128, :], xres[:, q, :], reads=yk, writes=[("ys", u)], key=("yst", q))

            gathers(0)
            gathers(1)
            for u in range(NU + 1):
                if u < NU:
                    front(u)
                if u >= 1:
                    back(u - 1)
                if u + 2 < NU:
                    gathers(u + 2)

        def combine_block(blk):
            tok0 = blk * TB
            yg = arena[:, 0:4096].rearrange("p (r c) -> p r c", r=4)
            for j in range(4):
                J = blk * 4 + j
                P.dma("sp", xres[:, j, :], x1_s[tok0 + j * 128:tok0 + (j + 1) * 128, :], reads=[("x1s", blk, j)], writes=[("x", j)], key=("x", j))
                for k in range(2):
                    r = (2 * j + k) % 4
                    gk = [[("abuf", 0), ("abuf", 1)], [("abuf", 2), ("abuf", 3)], [("cv", 0), ("cv", 1)], [("cv", 1), ("cv", 2), ("cv", 3)]][r]
                    P.add("pool", lambda e, r=r, k=k, J=J: e.indirect_dma_start(
                        out=yg[:, r, :], out_offset=None, in_=ys, in_offset=bass.IndirectOffsetOnAxis(ap=posi[:, k, J:J + 1], axis=0)),
                        reads=YSK + [("posi", k)], writes=gk, dma_key=("yg", r))
                    P.dve(lambda e, j=j, r=r, k=k, J=J: e.scalar_tensor_tensor(out=xres[:, j, :], in0=yg[:, r, :], scalar=srt[:, 3 + k, J:J + 1], in1=xres[:, j, :], op0=ALU.mult, op1=ALU.add),
                          reads=gk + [("x", j)] + SRTC, writes=[("x", j)])

        def ple_final(blk):
            tok0 = blk * TB
            rms_to_T(gple, "cols")
            P.dma("pool", pb[:], p_d[tok0:tok0 + TB, :].rearrange("(j p) c -> p j c", p=128), writes=["pb"], key="pb")
            b = PS.alloc()
            pv = ps[b][:].bitcast(BF16)
            for kk in range(2):
                for j in range(4):
                    P.pe(lambda e, kk=kk, j=j, pv=pv: e.transpose(out=pv[:, kk * 512 + j * 128:kk * 512 + (j + 1) * 128], in_=pb[:, j, kk * 128:(kk + 1) * 128], identity=idb[:]),
                         reads=["pb", "idb"], writes=[psk(b)])
            P.act(lambda e, pv=pv: e.activation(out=pT[:].rearrange("p a b -> p (a b)"), in_=pv, func=AF.Copy), reads=[psk(b)], writes=["pT"])
            PS.free(b)
            hple = None
            for hf in range(2):
                h0 = WS.get(("wpg", hf, 0))
                h1 = WS.get(("wpg", hf, 1))
                if hf == 0:
                    hple = WS.get(("wple",))
                for j in range(4):
                    b = PS.alloc()
                    for kt in range(8):
                        hcur = h0 if kt < 4 else h1
                        WS.check(hcur)
                        P.pe(lambda e, j=j, kt=kt, b=b, wv=hcur[1]: e.matmul(ps[b][:], lhsT=hT[:, kt, j * 128:(j + 1) * 128], rhs=wv[:, kt % 4, :],
                                                                          start=(kt == 0), stop=(kt == 7)), reads=[hcur[2]] + HTK, writes=[psk(b)])
                    WS.check(hple)
                    b2 = PS.alloc()
                    for kk in range(2):
                        P.pe(lambda e, j=j, hf=hf, kk=kk, b=b2, wv=hple[1]: e.matmul(ps[b][:], lhsT=pT[:, kk, j * 128:(j + 1) * 128], rhs=wv[:, kk, hf * 512:(hf + 1) * 512],
                                                                                  start=(kk == 0), stop=(kk == 1)), reads=[hple[2], "pT"], writes=[psk(b2)])
                    g = s5p[:, j % 2, :]
                    gk = ("s5p", j % 2)
                    tt(g, ps[b][:], bpgB[:, hf * 512:(hf + 1) * 512], ALU.add, [psk(b), "bpgB"], [gk])
                    P.act(lambda e, g=g: e.activation(out=g, in_=g, func=AF.Sigmoid), reads=[gk], writes=[gk])
                    tt(g, g, ps[b2][:], ALU.mult, [gk, psk(b2)], [gk])
                    xs = xres[:, j, hf * 512:(hf + 1) * 512]
                    tt(xs, xs, g, ALU.add, [("x", j), gk], [("x", j)])
                    PS.free(b)
                    PS.free(b2)
            rms_to_T(None, None, final=True)
            for j in range(4):
                P.dve(lambda e, j=j: e.scalar_tensor_tensor(out=ost[:], in0=xres[:, j, :], scalar=stat[:, 8 + j:9 + j], in1=gfinB[:], op0=ALU.mult, op1=ALU.mult),
                      reads=[("x", j), "rstd", "gfinB"], writes=["ost"])
                P.dma("sp", y_d[tok0 + j * 128:tok0 + (j + 1) * 128, :], ost[:], reads=["ost"], writes=[("y", j)], key=("y", j))


        def phase3():
            yg = arena[:, 0:4096].rearrange("p (r c) -> p r c", r=4)
            YGK = [[("abuf", 0), ("abuf", 1)], [("abuf", 2), ("abuf", 3)], [("cv", 0), ("cv", 1)], [("cv", 1), ("cv", 2), ("cv", 3)]]
            osts = [(ost[:], "ost"), (h2b[:, 0, :].bitcast(F32)[:, 0:512], None)]
            ost2 = h2b[:].rearrange("p a b -> p (a b)").bitcast(F32)
            osts = [(ost[:], ["ost"]), (ost2, [("h2b", 0), ("h2b", 1)])]
            hw = {}
            hw[(0, 0)] = WS.get(("wpg", 0, 0))
            hw[(0, 1)] = WS.get(("wpg", 0, 1))
            hple = WS.get(("wple",))
            hw[(1, 0)] = WS.get(("wpg", 1, 0))
            hw[(1, 1)] = WS.get(("wpg", 1, 1))

            def L(J):
                j = J % 4
                P.dma("sp", xres[:, j, :], x1_s[J * 128:(J + 1) * 128, :], reads=[("x1s", J // 4, j)], writes=[("x", j)], key=("x", j))
                for k in range(2):
                    r = (2 * J + k) % 4
                    P.add("pool", lambda e, r=r, k=k, J=J: e.indirect_dma_start(
                        out=yg[:, r, :], out_offset=None, in_=ys, in_offset=bass.IndirectOffsetOnAxis(ap=posi[:, k, J:J + 1], axis=0)),
                        reads=YSK + [("posi", k)], writes=YGK[r], dma_key=("yg", r))
                P.dma("pool", pb[:, j, :], p_d[J * 128:(J + 1) * 128, :], writes=[("pb", j)], key=("pb", j))

            def C(J):
                j = J % 4
                for k in range(2):
                    r = (2 * J + k) % 4
                    P.pool(lambda e, r=r, k=k, J=J: e.tensor_scalar(out=yg[:, r, :], in0=yg[:, r, :], scalar1=srt[:, 3 + k, J:J + 1], scalar2=None, op0=ALU.mult),
                           reads=YGK[r] + SRTC, writes=YGK[r])
                    P.pool(lambda e, j=j, r=r: e.tensor_tensor(out=xres[:, j, :], in0=xres[:, j, :], in1=yg[:, r, :], op=ALU.add),
                           reads=YGK[r] + [("x", j)], writes=[("x", j)])

            def rstd_tile(j, c0):
                P.act(lambda e, j=j: e.activation(out=junk3b[:], in_=xres[:, j, :], func=AF.Square, accum_out=st3[:, c0 + j:c0 + j + 1]),
                      reads=[("x", j)], writes=["junk3"] + [("st3", c0, j)])
                P.dve(lambda e, j=j: e.tensor_scalar(out=st3[:, c0 + 4 + j:c0 + 5 + j], in0=st3[:, c0 + j:c0 + j + 1], scalar1=1.0 / D, scalar2=1e-6, op0=ALU.mult, op1=ALU.add),
                      reads=[("st3", c0, j)], writes=[("st3b", c0, j)])
                P.act(lambda e, j=j: e.activation(out=st3[:, c0 + 4 + j:c0 + 5 + j], in_=st3[:, c0 + 4 + j:c0 + 5 + j], func=AF.Sqrt), reads=[("st3b", c0, j)], writes=[("st3b", c0, j)])
                P.dve(lambda e, j=j: e.reciprocal(out=st3[:, c0 + 8 + j:c0 + 9 + j], in_=st3[:, c0 + 4 + j:c0 + 5 + j]), reads=[("st3b", c0, j)], writes=[("st3r", c0, j)])

            def N(J):
                j = J % 4
                rstd_tile(j, 0)
                P.act(lambda e, j=j: e.activation(out=hn[:, j % 2, :], in_=xres[:, j, :], func=AF.Copy, scale=st3[:, 8 + j:9 + j]),
                      reads=[("x", j), ("st3r", 0, j)], writes=[("hn", j % 2)])
                b = PS.alloc()
                pv = ps[b][:].bitcast(BF16)
                for kt in range(8):
                    P.pe(lambda e, j=j, kt=kt, pv=pv: e.transpose(out=pv[:, kt * 128:(kt + 1) * 128], in_=hn[:, j % 2, kt * 128:(kt + 1) * 128], identity=idb[:]),
                         reads=[("hn", j % 2), "idb"], writes=[psk(b)])
                P.dve(lambda e, j=j, pv=pv: e.tensor_tensor(
                    out=hT[:, :, j * 128:(j + 1) * 128], in0=pv.rearrange("p (k t) -> p k t", k=8),
                    in1=gple.unsqueeze(2).to_broadcast([128, 8, 128]), op=ALU.mult),
                    reads=[psk(b), "cols"], writes=[("hT", j)])
                PS.free(b)
                b = PS.alloc()
                pv = ps[b][:].bitcast(BF16)
                for kk in range(2):
                    P.pe(lambda e, kk=kk, j=j, pv=pv: e.transpose(out=pv[:, kk * 128:(kk + 1) * 128], in_=pb[:, j, kk * 128:(kk + 1) * 128], identity=idb[:]),
                         reads=[("pb", j), "idb"], writes=[psk(b)])
                P.act(lambda e, pv=pv, j=j: e.activation(out=pT[:, :, j * 128:(j + 1) * 128], in_=pv[:, 0:256].rearrange("p (k t) -> p k t", k=2), func=AF.Copy),
                      reads=[psk(b)], writes=[("pT", j)])
                PS.free(b)

            def G(J):
                j = J % 4
                for hf in range(2):
                    b = PS.alloc()
                    for kt in range(8):
                        hcur = hw[(hf, kt // 4)]
                        P.pe(lambda e, j=j, kt=kt, b=b, wv=hcur[1]: e.matmul(ps[b][:], lhsT=hT[:, kt, j * 128:(j + 1) * 128], rhs=wv[:, kt % 4, :],
                                                                          start=(kt == 0), stop=(kt == 7)), reads=[hcur[2], ("hT", j)], writes=[psk(b)])
                    b2 = PS.alloc()
                    for kk in range(2):
                        P.pe(lambda e, j=j, hf=hf, kk=kk, b=b2, wv=hple[1]: e.matmul(ps[b][:], lhsT=pT[:, kk, j * 128:(j + 1) * 128], rhs=wv[:, kk, hf * 512:(hf + 1) * 512],
                                                                                  start=(kk == 0), stop=(kk == 1)), reads=[hple[2], ("pT", j)], writes=[psk(b2)])
                    g = s5p[:, hf, :]
                    gk = ("s5p", hf)
                    tt(g, ps[b][:], bpgB[:, hf * 512:(hf + 1) * 512], ALU.add, [psk(b), "bpgB"], [gk])
                    P.act(lambda e, g=g: e.activation(out=g, in_=g, func=AF.Sigmoid), reads=[gk], writes=[gk])
                    tt(g, g, ps[b2][:], ALU.mult, [gk, psk(b2)], [gk])
                    xs = xres[:, j, hf * 512:(hf + 1) * 512]
                    tt(xs, xs, g, ALU.add, [("x", j), gk], [("x", j)])
                    PS.free(b)
                    PS.free(b2)

            def Fin(J):
                j = J % 4
                rstd_tile(j, 16)
                oap, okeys = osts[J % 2]
                P.dve(lambda e, j=j, oap=oap: e.scalar_tensor_tensor(out=oap, in0=xres[:, j, :], scalar=st3[:, 24 + j:25 + j], in1=gfinB[:], op0=ALU.mult, op1=ALU.mult),
                      reads=[("x", j), ("st3r", 16, j), "gfinB"], writes=okeys)
                P.dma("sp", y_d[J * 128:(J + 1) * 128, :], oap, reads=okeys, writes=[("y", J % 4)], key=("y", J % 4))

            for it in range(NJ + 3):
                if 0 <= it - 2 < NJ:
                    C(it - 2)
                if it < NJ:
                    L(it)
                if 0 <= it - 3 < NJ:
                    G(it - 3)
                if 0 <= it - 2 < NJ:
                    N(it - 2)
                if 0 <= it - 3 < NJ:
                    Fin(it - 3)

        for blk in range(nblk):
            mixer_block(blk)
            route_block(blk)
        emit_prepass(1000)
        sort_phase()
        scatter_phase()
        unit_phase()
        phase3()

        P.add("sp", None, reads=[("y", j) for j in range(4)])
        P.finalize()
        info = dict(counts=P.counts, nsem=P.nsem, maxcnt=P.maxcnt)
    return nc, info


_WNAMES = ["norm_mix", "w_in", "b_in", "ssm_lam_re", "ssm_lam_im", "ssm_log_dt", "ssm_b_re", "ssm_b_im", "ssm_c_re",
           "ssm_c_im", "ssm_d", "w_glu_a", "w_glu_b", "conv_w", "conv_b", "w_conv_out", "w_o", "norm_ffn",
           "w_router_group", "b_router_group", "w_router_expert", "b_router_expert", "w_exp_gate", "w_exp_up",
           "w_exp_down", "norm_ple", "w_ple", "w_ple_gate", "b_ple_gate"]


def make_in_maps(inputs, ncores=NCORES):
    x = np.ascontiguousarray(np.asarray(inputs["x"], dtype=np.float32)).reshape(-1, D)
    p = np.ascontiguousarray(np.asarray(inputs["p"], dtype=np.float32)).reshape(-1, 256)
    shared = {k: np.ascontiguousarray(np.asarray(inputs[k], dtype=np.float32)[0]) for k in _WNAMES}
    shared["norm_final"] = np.ascontiguousarray(np.asarray(inputs["norm_final"], dtype=np.float32))
    maps = []
    for c in range(ncores):
        m = dict(shared)
        m["x"] = x[c * TOK:(c + 1) * TOK]
        m["p"] = p[c * TOK:(c + 1) * TOK]
        maps.append(m)
    return maps


def kernel(**inputs):
    nc, _ = build_program()
    in_maps = make_in_maps(inputs)
    res = run_bass_kernel_spmd(nc, in_maps, core_ids=list(range(NCORES)))
    out = np.concatenate([np.asarray(r["y"]) for r in res.results], axis=0)
    return out.reshape(16, SEQ, D).astype(np.float32)
```

```python
import math
from contextlib import ExitStack

import numpy as np
import concourse.bass as bass
import concourse.mybir as mybir
from concourse.bass_utils import run_bass_kernel_spmd

F32 = mybir.dt.float32
BF16 = mybir.dt.bfloat16
I32 = mybir.dt.int32
AF = mybir.ActivationFunctionType
ALU = mybir.AluOpType
AX = mybir.AxisListType

NCORES = 8
SEQ = 2048
TOK = 4096
D = 1024
TB = 512
NBLK = TOK // TB
CH = 128
NS = 8
DEPTH = 4
PI = math.pi
BIG = 1.0e9
SAME_ENGINE_FREE = False


class Op:
    __slots__ = ("eng", "fn", "reads", "writes", "dma_key", "deps", "inc", "cnt", "waits", "dma_val")

    def __init__(self, eng, fn, reads, writes, dma_key):
        self.eng = eng
        self.fn = fn
        self.reads = tuple(reads)
        self.writes = tuple(writes)
        self.dma_key = dma_key
        self.inc = False
        self.cnt = 0
        self.waits = []
        self.dma_val = 0


class Prog:
    ENG = ("pe", "act", "dve", "pool", "sp")

    def __init__(self, nc, es):
        self.nc = nc
        self.es = es
        self.ops = []

    def sb(self, name, shape, dtype):
        self.sbytes = getattr(self, "sbytes", 0) + int(np.prod(shape[1:])) * mybir.dt.size(dtype)
        return self.es.enter_context(self.nc.sbuf_tensor(name, list(shape), dtype))

    def ps(self, name, shape, dtype):
        return self.es.enter_context(self.nc.psum_tensor(name, list(shape), dtype))

    def add(self, eng, fn, reads=(), writes=(), dma_key=None):
        self.ops.append(Op(eng, fn, reads, writes, dma_key))

    def pe(self, fn, reads=(), writes=()):
        self.add("pe", fn, reads, writes)

    def act(self, fn, reads=(), writes=()):
        self.add("act", fn, reads, writes)

    def dve(self, fn, reads=(), writes=()):
        self.add("dve", fn, reads, writes)

    def pool(self, fn, reads=(), writes=()):
        self.add("pool", fn, reads, writes)

    def dma(self, eng, out, in_, reads=(), writes=(), key=None, **kw):
        assert key is not None
        self.add(eng, lambda e: e.dma_start(out=out, in_=in_, **kw), reads, writes, dma_key=key)

    def finalize(self):
        nc, es, ops = self.nc, self.es, self.ops
        last_w, readers, dma_cnt = {}, {}, {}
        for i, op in enumerate(ops):
            deps = set()
            for r in op.reads:
                if r in last_w:
                    deps.add(last_w[r])
            for w in op.writes:
                if w in last_w:
                    deps.add(last_w[w])
                deps.update(readers.get(w, ()))
            deps.discard(i)
            op.deps = deps
            for r in op.reads:
                readers.setdefault(r, []).append(i)
            for w in op.writes:
                last_w[w] = i
                readers[w] = []
            if op.dma_key is not None:
                dma_cnt[op.dma_key] = dma_cnt.get(op.dma_key, 0) + 1
                op.dma_val = 16 * dma_cnt[op.dma_key]

        def skip(pj, op):
            if pj.eng == "pe" and op.eng == "pe":
                return True
            return SAME_ENGINE_FREE and pj.eng == op.eng and pj.eng in ("dve",) and op.dma_key is None

        for op in ops:
            for j in op.deps:
                pj = ops[j]
                if pj.dma_key is None and not skip(pj, op):
                    pj.inc = True
        cnt = {e: 0 for e in self.ENG}
        for op in ops:
            if op.inc:
                cnt[op.eng] += 1
                op.cnt = cnt[op.eng]
        self.ndma = len(dma_cnt)
        print("n dma keys", len(dma_cnt), "sbuf KiB/partition", self.sbytes / 1024)
        esem = {e: es.enter_context(nc.semaphore("s_" + e)) for e in self.ENG}
        dsem = {}
        for k in dma_cnt:
            dsem[k] = es.enter_context(nc.semaphore("d%d" % len(dsem)))
        self.nsem = len(esem) + len(dsem)
        waited = {e: {} for e in self.ENG}
        for op in ops:
            w = {}
            for j in op.deps:
                pj = ops[j]
                if pj.dma_key is not None:
                    sid, val = ("d", pj.dma_key), pj.dma_val
                else:
                    if skip(pj, op):
                        continue
                    sid, val = ("e", pj.eng), pj.cnt
                if val > w.get(sid, 0):
                    w[sid] = val
            wd = waited[op.eng]
            for sid, val in w.items():
                if val > wd.get(sid, 0):
                    wd[sid] = val
                    sem = dsem[sid[1]] if sid[0] == "d" else esem[sid[1]]
                    op.waits.append((sem, val))
        per = {e: [op for op in ops if op.eng == e] for e in self.ENG}
        self.counts = {e: len(per[e]) for e in self.ENG}
        self.maxcnt = dict(cnt)

        def replay(name, eng):
            for op in per[name]:
                for sem, val in op.waits:
                    eng.wait_ge(sem, val)
                if op.fn is None:
                    continue
                inst = op.fn(eng)
                if op.dma_key is not None:
                    inst.then_inc(dsem[op.dma_key], 16)
                elif op.inc:
                    inst.then_inc(esem[name], 1)

        with nc.Block() as block:
            @block.tensor
            def _(e):
                replay("pe", e)

            @block.scalar
            def _(e):
                replay("act", e)

            @block.vector
            def _(e):
                replay("dve", e)

            @block.gpsimd
            def _(e):
                replay("pool", e)

            @block.sync
            def _(e):
                replay("sp", e)


class PsumAlloc:
    def __init__(self, P):
        self.t = [P.ps("psb%d" % i, [128, 512], F32) for i in range(8)]
        self.free_list = list(range(8))

    def alloc(self):
        assert self.free_list, "PSUM exhausted"
        return self.free_list.pop(0)

    def free(self, b):
        assert b not in self.free_list
        self.free_list.append(b)


class WStream:
    def __init__(self, P, ring, order):
        self.P = P
        self.ring = ring
        self.order = order
        self.next_dma = 0
        self.next_get = 0
        self.dead = -1

    def slot_view(self, k, shape):
        s = k % NS
        v = self.ring[:, s * 2048:(s + 1) * 2048]
        if len(shape) == 2:
            return v.rearrange("p (a b) -> p a b", a=shape[0])
        return v

    def get(self, tag):
        k = self.next_get
        assert self.order[k][0] == tag, (self.order[k][0], tag)
        self.next_get += 1
        lim = min(len(self.order), k + DEPTH + 1)
        while self.next_dma < lim:
            j = self.next_dma
            _, src, shape, rkeys = self.order[j]
            self.P.dma("sp", self.slot_view(j, shape), src, reads=rkeys, writes=[("w", j % NS)], key=("w", j % NS))
            self.dead = max(self.dead, j - NS)
            self.next_dma += 1
        return (k, self.slot_view(k, self.order[k][2]), ("w", k % NS))

    def check(self, h):
        assert h[0] > self.dead, "weight chunk overwritten before use"


def build_program(nblk=NBLK, stop="full"):
    assert stop == "full"
    nc = bass.Bass("TRN2", target_bir_lowering=False)

    def din(name, shape):
        return nc.dram_tensor(name, list(shape), F32, kind="ExternalInput").ap()

    def dscr(name, shape):
        return nc.dram_tensor(name, list(shape), BF16, kind="Internal").ap()

    x_d = din("x", [TOK, D])
    p_d = din("p", [TOK, 256])
    norm_mix = din("norm_mix", [D])
    w_in = din("w_in", [D, 4096])
    b_in = din("b_in", [4096])
    lam_re = din("ssm_lam_re", [32, 64])
    lam_im = din("ssm_lam_im", [32, 64])
    log_dt = din("ssm_log_dt", [32])
    sb_re = din("ssm_b_re", [32, 64, 16])
    sb_im = din("ssm_b_im", [32, 64, 16])
    sc_re = din("ssm_c_re", [32, 16, 64])
    sc_im = din("ssm_c_im", [32, 16, 64])
    ssm_d = din("ssm_d", [512])
    w_glu_a = din("w_glu_a", [512, D])
    w_glu_b = din("w_glu_b", [512, D])
    conv_w = din("conv_w", [3, 512])
    conv_b = din("conv_b", [512])
    w_conv_out = din("w_conv_out", [512, D])
    w_o = din("w_o", [D, D])
    norm_ffn = din("norm_ffn", [D])
    w_rg = din("w_router_group", [D, 4])
    b_rg = din("b_router_group", [4])
    w_re = din("w_router_expert", [D, 32])
    b_re = din("b_router_expert", [32])
    w_eg = din("w_exp_gate", [32, D, 256])
    w_eu = din("w_exp_up", [32, D, 256])
    w_ed = din("w_exp_down", [32, 256, D])
    norm_ple = din("norm_ple", [D])
    w_ple = din("w_ple", [256, D])
    w_pg = din("w_ple_gate", [D, D])
    b_pg = din("b_ple_gate", [D])
    norm_final = din("norm_final", [D])
    y_d = nc.dram_tensor("y", [TOK, D], F32, kind="ExternalOutput").ap()

    win_s = dscr("win_s", [D, 4096])
    glua_s = dscr("glua_s", [512, D])
    glub_s = dscr("glub_s", [512, D])
    wco_s = dscr("wco_s", [512, D])
    wo_s = dscr("wo_s", [D, D])
    wpg_s = dscr("wpg_s", [D, D])
    wple_s = dscr("wple_s", [256, D])
    wgp = dscr("wgp", [32 * 128, 2048])
    wup = dscr("wup", [32 * 128, 2048])
    wdp = dscr("wdp", [32 * 128, 2048])
    h2_s = dscr("h2_s", [TOK, D])
    hs = dscr("hs", [96 * 128, D])
    x1_s = nc.dram_tensor("x1_s", [TOK, D], F32, kind="Internal").ap()
    ys = nc.dram_tensor("ys", [96 * 128, D], F32, kind="Internal").ap()

    with ExitStack() as es:
        P = Prog(nc, es)
        PS = PsumAlloc(P)
        ps = PS.t

        def psk(b):
            return ("ps", b)

        xres = P.sb("xres", [128, 4, D], F32)
        hT = P.sb("hT", [128, 8, TB], BF16)
        hn = P.sb("hn", [128, 2, D], BF16)
        stat = P.sb("stat", [128, 16], F32)
        uT = P.sb("uT", [128, 4, TB], BF16)
        s5t = P.sb("s5t", [128, 4, TB], F32)
        sri = P.sb("sri", [128, 2, 2, TB], BF16)
        wlast = P.sb("wlast", [128, 16, 2], F32)
        chn = P.sb("chn", [128, 2, 4], F32)
        s5u = P.sb("s5u", [128, 4, TB], F32)
        s5p = P.sb("s5p", [128, 2, TB], F32)
        COS = P.sb("COS", [128, 16, CH], F32)
        SIN = P.sb("SIN", [128, 16, CH], F32)
        LBk = [P.sb("LBk%d" % i, [128, 4, 4, 128], BF16) for i in range(2)]
        LCk = [P.sb("LCk%d" % i, [128, 4, 16, 32], BF16) for i in range(2)]
        diagD = P.sb("diagD", [128, 4, 128], BF16)
        cols = P.sb("cols", [128, 108], F32)
        stg = P.sb("stg", [128, 128], F32)
        sc = P.sb("sc", [128, 24, 16], F32)
        bnat = [P.sb("bnat%d" % i, [128, 16, 16], F32) for i in range(2)]
        btmp = [P.sb("btmp%d" % i, [128, 16, 16], F32) for i in range(4)]
        cnat = [P.sb("cnat%d" % i, [128, 4, 64], F32) for i in range(2)]
        cmk = P.sb("cmk", [128, 4, 8], F32)
        ybf = P.sb("ybf", [128, 4, TB], BF16)
        arena = P.sb("arena", [128, 4112], F32)
        abuf = arena[:, 0:2048].rearrange("p (a b) -> p a b", a=4)
        cvbuf = arena[:, 2048:2048 + 4 * (TB + 2)].rearrange("p (a b) -> p a b", a=4)
        gbT = P.sb("gbT", [128, 4, TB], BF16)
        mixT = P.sb("mixT", [128, 8, TB], BF16)
        lsb = P.sb("lsb", [128, 4, 36], F32)
        brB = P.sb("brB", [128, 36], F32)
        wr = P.sb("wr", [128, 8, 36], BF16)
        pb = P.sb("pb", [128, 4, 256], BF16)
        pT = P.sb("pT", [128, 2, TB], BF16)
        bpgB = P.sb("bpgB", [128, D], F32)
        gfinB = P.sb("gfinB", [128, D], F32)
        ost = P.sb("ost", [128, D], F32)
        junk = ost[:].bitcast(BF16)[:, 0:D]
        idf = P.sb("idf", [128, 128], F32)
        idb = P.sb("idb", [128, 128], BF16)
        idi = P.sb("idi", [128, 128], F32)
        ring = P.sb("ring", [128, NS * 2048], BF16)

        XK = [("x", j) for j in range(4)]
        S5K = [("s5t", i) for i in range(4)]
        CVK = [("cv", m) for m in range(4)]

        def flat2(ap, rows):
            return ap.rearrange("(a b) c -> a (b c)", a=rows)

        for cc in (0, 2, 3, 1, 4, 5, 6, 7):
            P.dma("pool", win_s[:, cc * 512:(cc + 1) * 512], w_in[:, cc * 512:(cc + 1) * 512], writes=[("win_s", cc)], key=("win_s", cc))
        for nm, dst, src in (("glua_s", glua_s, w_glu_a), ("glub_s", glub_s, w_glu_b), ("wco_s", wco_s, w_conv_out),
                             ("wo_s", wo_s, w_o)):
            P.dma("pool", flat2(dst, 128), flat2(src, 128), writes=[nm], key=nm)
        prepass_q = []

        def emit_prepass(n):
            for _ in range(min(n, len(prepass_q))):
                d_, s_, wk_, key_ = prepass_q.pop(0)
                P.dma("pool", d_, s_, writes=[wk_], key=key_)

        for i, (dst, src, kk) in enumerate(((wgp, w_eg, 8), (wup, w_eu, 8), (wdp, w_ed, 2))):
            for g in range(4):
                d4 = dst[g * 1024:(g + 1) * 1024, :].rearrange("(e p) (k c) -> e k p c", p=128, k=kk)
                s4 = src[8 * g:8 * g + 8].rearrange("e (k p) c -> e k p c", p=128)
                for e8 in range(8):
                    prepass_q.append((d4[e8], s4[e8], ("wpermw", i, g, e8), ("wperm", i)))
        prepass_q.sort(key=lambda q: (q[2][2], q[2][3], q[2][1]))
        WPK = {i: [("wpermw", i, g, e8) for g in range(4) for e8 in range(8)] for i in range(3)}
        for nm, dst, src in (("wpg_s", wpg_s, w_pg), ("wple_s", wple_s, w_ple)):
            P.dma("pool", flat2(dst, 128), flat2(src, 128), writes=[nm], key=nm)

        def kt_view(ap2d):
            return ap2d.rearrange("(kt p) n -> p kt n", p=128)

        order = []
        for blk in range(nblk):
            def win(c):
                order.append((("win", c), kt_view(win_s)[:, :, c * 256:(c + 1) * 256], (8, 256), [("win_s", c // 2)]))
            for c in (0, 1, 4, 5, 6, 7, 2, 3):
                win(c)
            for h in range(2):
                order.append((("glua", h), kt_view(glua_s)[:, :, h * 512:(h + 1) * 512], (4, 512), ["glua_s"]))
                order.append((("glub", h), kt_view(glub_s)[:, :, h * 512:(h + 1) * 512], (4, 512), ["glub_s"]))
                win(8 + 2 * h)
                win(9 + 2 * h)
            for h in range(2):
                order.append((("wco", h), kt_view(wco_s)[:, :, h * 512:(h + 1) * 512], (4, 512), ["wco_s"]))
                win(12 + 2 * h)
                win(13 + 2 * h)
            for hf in range(2):
                for kh in range(2):
                    order.append((("wo", hf, kh), kt_view(wo_s)[:, 4 * kh:4 * kh + 4, hf * 512:(hf + 1) * 512], (4, 512), ["wo_s"]))
        for hf in range(2):
            for kh in range(2):
                order.append((("wpg", hf, kh), kt_view(wpg_s)[:, 4 * kh:4 * kh + 4, hf * 512:(hf + 1) * 512], (4, 512), ["wpg_s"]))
            if hf == 0:
                order.append((("wple",), kt_view(wple_s), (2, 1024), ["wple_s"]))
        WS = WStream(P, ring, order)

        P.pool(lambda e: e.iota(idi[:], pattern=[[1, 128]], base=0, channel_multiplier=-1,
                                allow_small_or_imprecise_dtypes=True), writes=["idi"])
        P.dve(lambda e: e.tensor_single_scalar(out=idf[:], in_=idi[:], scalar=0.0, op=ALU.is_equal), reads=["idi"], writes=["idf"])
        P.dve(lambda e: e.tensor_copy(out=idb[:], in_=idf[:]), reads=["idf"], writes=["idb"])
        P.pool(lambda e: e.iota(cmk[:, 0, :], pattern=[[-16, 8]], base=0, channel_multiplier=1,
                                allow_small_or_imprecise_dtypes=True), writes=["cmk0"])
        P.dve(lambda e: e.tensor_single_scalar(out=cmk[:, 1, :], in_=cmk[:, 0, :], scalar=0.0, op=ALU.is_ge), reads=["cmk0"], writes=["cmk1"])
        P.dve(lambda e: e.tensor_single_scalar(out=cmk[:, 2, :], in_=cmk[:, 0, :], scalar=15.0, op=ALU.is_le), reads=["cmk0"], writes=["cmk2"])
        P.dve(lambda e: e.tensor_tensor(out=cmk[:, 2, :], in0=cmk[:, 2, :], in1=cmk[:, 1, :], op=ALU.mult), reads=["cmk1", "cmk2"], writes=["cmk2"])
        P.dve(lambda e: e.tensor_scalar(out=cmk[:, 3, :], in0=cmk[:, 2, :], scalar1=-1.0, scalar2=None, op0=ALU.mult), reads=["cmk2"], writes=["cmk3"])

        P.dve(lambda e: e.memset(stg[:], 0.0), writes=["stg"])
        rows = [(norm_mix, 0, 8), (norm_ffn, 8, 8), (norm_ple, 16, 8), (b_in, 24, 32), (ssm_d, 56, 4),
                (conv_w.rearrange("k c -> (k c)"), 60, 12), (conv_b, 72, 4),
                (lam_re.rearrange("g p -> (g p)"), 76, 16), (lam_im.rearrange("g p -> (g p)"), 92, 16)]
        for i, (src, r0, n) in enumerate(rows):
            P.dma("sp", stg[r0:r0 + n, :], src.rearrange("(a b) -> a b", b=128), reads=["stg"], writes=[("stgr", i)], key=("stgr", i))
        b0 = PS.alloc()
        P.pe(lambda e: e.transpose(out=ps[b0][:, 0:128], in_=stg[:, :], identity=idf[:]),
             reads=["idf", "stg"] + [("stgr", i) for i in range(len(rows))], writes=[psk(b0)])
        P.dve(lambda e: e.tensor_copy(out=cols[:], in_=ps[b0][:, 0:108]), reads=[psk(b0)], writes=["cols"])
        PS.free(b0)
        gmix, gffn, gple = cols[:, 0:8], cols[:, 8:16], cols[:, 16:24]
        binc, dcol, cw, cbc = cols[:, 24:56], cols[:, 56:60], cols[:, 60:72], cols[:, 72:76]
        lre, lim = cols[:, 76:92], cols[:, 92:108]

        P.dma("sp", bpgB[:], b_pg.partition_broadcast(128), writes=["bpgB"], key="bpgB")
        P.dma("sp", gfinB[:], norm_final.partition_broadcast(128), writes=["gfinB"], key="gfinB")
        P.dma("sp", brB[:, 0:4], b_rg.partition_broadcast(128), writes=["brB0"], key="brB0")
        P.dma("sp", brB[:, 4:36], b_re.partition_broadcast(128), writes=["brB1"], key="brB1")
        BRB = ["brB0", "brB1"]
        P.dma("pool", wr[:, :, 0:4], kt_view(w_rg), writes=["wr0"], key="wr0")
        P.dma("pool", wr[:, :, 4:36], kt_view(w_re), writes=["wr1"], key="wr1")
        WRK = ["wr0", "wr1"]

        def S(i):
            return sc[:, i, :]

        def sk(i):
            return ("sc", i)

        ldv = log_dt.rearrange("(s g) -> g s", g=2)
        for g2 in range(2):
            P.dma("sp", sc[g2 * 64:(g2 + 1) * 64, 0, :], ldv[g2].partition_broadcast(64), writes=[("ldt", g2)],
                  key=("ldt", g2), allow_slow_non_contiguous=True)
        LDT = [("ldt", 0), ("ldt", 1)]

        def tt(out, a, b, op, reads, writes):
            P.dve(lambda e: e.tensor_tensor(out=out, in0=a, in1=b, op=op), reads=reads, writes=writes)

        def wrap(ap, tmp, rk, tk, n=1):
            for _ in range(n):
                P.dve(lambda e: e.tensor_scalar(out=tmp, in0=ap, scalar1=PI, scalar2=2 * PI, op0=ALU.is_gt, op1=ALU.mult), reads=rk, writes=tk)
                P.dve(lambda e: e.tensor_tensor(out=ap, in0=ap, in1=tmp, op=ALU.subtract), reads=rk + tk, writes=rk)
                P.dve(lambda e: e.tensor_scalar(out=tmp, in0=ap, scalar1=-PI, scalar2=2 * PI, op0=ALU.is_lt, op1=ALU.mult), reads=rk, writes=tk)
                P.dve(lambda e: e.tensor_tensor(out=ap, in0=ap, in1=tmp, op=ALU.add), reads=rk + tk, writes=rk)

        P.act(lambda e: e.activation(out=S(0), in_=S(0), func=AF.Exp), reads=LDT, writes=[sk(0)])
        tt(S(1), lre, S(0), ALU.mult, ["cols", sk(0)], [sk(1)])
        P.act(lambda e: e.activation(out=S(1), in_=S(1), func=AF.Exp), reads=[sk(1)], writes=[sk(1)])
        tt(S(2), lim, S(0), ALU.mult, ["cols", sk(0)], [sk(2)])
        wrap(S(2), S(10), [sk(2)], [sk(10)], n=8)
        P.act(lambda e: e.activation(out=S(3), in_=S(2), func=AF.Sin), reads=[sk(2)], writes=[sk(3)])
        P.dve(lambda e: e.tensor_scalar(out=S(11), in0=S(2), scalar1=PI / 2, scalar2=None, op0=ALU.add), reads=[sk(2)], writes=[sk(11)])
        wrap(S(11), S(10), [sk(11)], [sk(10)])
        P.act(lambda e: e.activation(out=S(4), in_=S(11), func=AF.Sin), reads=[sk(11)], writes=[sk(4)])
        tt(S(5), S(1), S(4), ALU.mult, [sk(1), sk(4)], [sk(5)])
        tt(S(6), S(1), S(3), ALU.mult, [sk(1), sk(3)], [sk(6)])
        P.dve(lambda e: e.tensor_scalar(out=S(5), in0=S(5), scalar1=-1.0, scalar2=None, op0=ALU.add), reads=[sk(5)], writes=[sk(5)])
        tt(S(7), lre, lre, ALU.mult, ["cols"], [sk(7)])
        tt(S(10), lim, lim, ALU.mult, ["cols"], [sk(10)])
        tt(S(7), S(7), S(10), ALU.add, [sk(7), sk(10)], [sk(7)])
        P.dve(lambda e: e.reciprocal(out=S(7), in_=S(7)), reads=[sk(7)], writes=[sk(7)])
        tt(S(8), S(5), lre, ALU.mult, [sk(5), "cols"], [sk(8)])
        tt(S(10), S(6), lim, ALU.mult, [sk(6), "cols"], [sk(10)])
        tt(S(8), S(8), S(10), ALU.add, [sk(8), sk(10)], [sk(8)])
        tt(S(8), S(8), S(7), ALU.mult, [sk(8), sk(7)], [sk(8)])
        tt(S(9), S(6), lre, ALU.mult, [sk(6), "cols"], [sk(9)])
        tt(S(10), S(5), lim, ALU.mult, [sk(5), "cols"], [sk(10)])
        tt(S(9), S(9), S(10), ALU.subtract, [sk(9), sk(10)], [sk(9)])
        tt(S(9), S(9), S(7), ALU.mult, [sk(9), sk(7)], [sk(9)])

        xflat = xres[:].rearrange("p a b -> p (a b)")
        ANG = xflat[:, 0:CH * 16]
        TMPW = xflat[:, CH * 16:2 * CH * 16]
        s5flat = s5t[:].rearrange("p a b -> p (a b)")
        P.dve(lambda e: e.memset(ANG[:, 0:16], 0.0), writes=XK)
        P.dve(lambda e: e.tensor_copy(out=S(12), in_=S(2)), reads=[sk(2)], writes=[sk(12)])
        n = 1
        while n < CH:
            dst = ANG[:, n * 16:2 * n * 16]
            src = ANG[:, 0:n * 16]
            P.dve(lambda e, dst=dst, src=src, n=n: e.tensor_tensor(
                out=dst.rearrange("p (j s) -> p j s", s=16), in0=src.rearrange("p (j s) -> p j s", s=16),
                in1=S(12).unsqueeze(1).to_broadcast([128, n, 16]), op=ALU.add), reads=XK + [sk(12)], writes=XK)
            wrap(dst, s5flat[:, 0:n * 16], XK, S5K)
            P.dve(lambda e: e.tensor_scalar(out=S(12), in0=S(12), scalar1=2.0, scalar2=None, op0=ALU.mult), reads=[sk(12)], writes=[sk(12)])
            wrap(S(12), S(10), [sk(12)], [sk(10)])
            n *= 2
        P.dve(lambda e: e.tensor_scalar(out=S(15), in0=S(12), scalar1=2.0, scalar2=None, op0=ALU.mult), reads=[sk(12)], writes=[sk(15)])
        wrap(S(15), S(10), [sk(15)], [sk(10)])
        tt(S(16), S(15), S(12), ALU.add, [sk(15), sk(12)], [sk(16)])
        wrap(S(16), S(10), [sk(16)], [sk(10)])
        P.dve(lambda e: e.tensor_scalar(out=S(17), in0=S(15), scalar1=2.0, scalar2=None, op0=ALU.mult), reads=[sk(15)], writes=[sk(17)])
        wrap(S(17), S(10), [sk(17)], [sk(10)])

        def cossin(pi_, ci_, si_):
            P.act(lambda e: e.activation(out=S(si_), in_=S(pi_), func=AF.Sin), reads=[sk(pi_)], writes=[sk(si_)])
            P.dve(lambda e: e.tensor_scalar(out=S(11), in0=S(pi_), scalar1=PI / 2, scalar2=None, op0=ALU.add), reads=[sk(pi_)], writes=[sk(11)])
            wrap(S(11), S(10), [sk(11)], [sk(10)])
            P.act(lambda e: e.activation(out=S(ci_), in_=S(11), func=AF.Sin), reads=[sk(11)], writes=[sk(ci_)])
        cossin(12, 18, 19)
        cossin(15, 20, 21)
        cossin(16, 22, 23)
        cossin(17, 13, 14)
        P.act(lambda e: e.activation(out=SIN[:].rearrange("p s c -> p c s"), in_=ANG.rearrange("p (j s) -> p j s", s=16), func=AF.Sin),
              reads=XK, writes=["SIN"])
        P.dve(lambda e: e.tensor_scalar(out=TMPW, in0=ANG, scalar1=PI / 2, scalar2=None, op0=ALU.add), reads=XK, writes=XK)
        wrap(TMPW, s5flat[:, 0:CH * 16], XK, S5K)
        P.act(lambda e: e.activation(out=COS[:].rearrange("p s c -> p c s"), in_=TMPW.rearrange("p (j s) -> p j s", s=16), func=AF.Sin),
              reads=XK, writes=["COS"])

        ringF = ring[:].bitcast(F32)
        A = [ringF[:, 0:2048].rearrange("p (s c) -> p s c", s=16), ringF[:, 2048:4096].rearrange("p (s c) -> p s c", s=16)]
        AK = [[("w", 0), ("w", 1)], [("w", 2), ("w", 3)]]
        P.dma("sp", bnat[0][:], sb_re.rearrange("(s g) p h -> (g p) s h", g=2), writes=["bnat0"], key="bnat0")
        P.dma("sp", bnat[1][:], sb_im.rearrange("(s g) p h -> (g p) s h", g=2), writes=["bnat1"], key="bnat1")
        P.dma("sp", cnat[0][:], sc_re.rearrange("(m g) h p -> (g h) m p", g=8), writes=["cnat0"], key="cnat0")
        P.dma("sp", cnat[1][:], sc_im.rearrange("(m g) h p -> (g h) m p", g=8), writes=["cnat1"], key="cnat1")
        fre_b = S(8).unsqueeze(2).to_broadcast([128, 16, 16])
        fim_b = S(9).unsqueeze(2).to_broadcast([128, 16, 16])
        tt(btmp[0][:], bnat[0][:], fre_b, ALU.mult, ["bnat0", sk(8)], ["bt0"])
        tt(btmp[1][:], bnat[1][:], fim_b, ALU.mult, ["bnat1", sk(9)], ["bt1"])
        tt(btmp[2][:], bnat[1][:], fre_b, ALU.mult, ["bnat1", sk(8)], ["bt2"])
        tt(btmp[3][:], bnat[0][:], fim_b, ALU.mult, ["bnat0", sk(9)], ["bt3"])
        for i in range(2):
            P.dve(lambda e, i=i: e.memset(ringF[:, 2048 * i:2048 * (i + 1)], 0.0), writes=AK[i])
        tt(btmp[0][:], btmp[0][:], btmp[1][:], ALU.subtract, ["bt0", "bt1"], ["bt0"])
        tt(btmp[2][:], btmp[2][:], btmp[3][:], ALU.add, ["bt2", "bt3"], ["bt2"])

        def transposes(i, evac):
            for t in (3, 2, 1, 0):
                b = PS.alloc()
                for m_ in range(4):
                    s = 4 * m_ + t
                    P.pe(lambda e, s=s, m_=m_, b=b: e.transpose(out=ps[b][:, m_ * 128:(m_ + 1) * 128], in_=A[i][:, s, :], identity=idf[:]),
                         reads=AK[i] + ["idf"], writes=[psk(b)])
                evac(t, b)
                PS.free(b)

        for c in range(4):
            if c == 0:
                srcs = ((btmp[0], "bt0"), (btmp[2], "bt2"))
            else:
                a_b = S(16 + 2 * c).unsqueeze(2).to_broadcast([128, 16, 16])
                b_b = S(17 + 2 * c).unsqueeze(2).to_broadcast([128, 16, 16])
                ak, bk = sk(16 + 2 * c), sk(17 + 2 * c)
                tt(bnat[0][:], btmp[0][:], a_b, ALU.mult, ["bt0", ak, "bnat0"], ["bnat0"])
                tt(bnat[1][:], btmp[2][:], b_b, ALU.mult, ["bt2", bk, "bnat1"], ["bnat1"])
                tt(btmp[1][:], bnat[0][:], bnat[1][:], ALU.add, ["bnat0", "bnat1"], ["bt1"])
                tt(bnat[0][:], btmp[2][:], a_b, ALU.mult, ["bt2", ak, "bnat0"], ["bnat0"])
                tt(bnat[1][:], btmp[0][:], b_b, ALU.mult, ["bt0", bk, "bnat1"], ["bnat1"])
                tt(btmp[3][:], bnat[0][:], bnat[1][:], ALU.subtract, ["bnat0", "bnat1"], ["bt3"])
                srcs = ((btmp[1], "bt1"), (btmp[3], "bt3"))
            for i, (src, skey) in enumerate(srcs):
                for q_ in range(4):
                    for g2 in range(2):
                        c0 = q_ * 32 + g2 * 16
                        pr = slice(g2 * 64, (g2 + 1) * 64)
                        if i == 0:
                            P.dve(lambda e, q_=q_, pr=pr, c0=c0, src=src: e.tensor_copy(out=A[0][pr, q_:16:4, c0:c0 + 16], in_=src[pr, q_:16:4, :]), reads=[skey], writes=AK[0])
                        else:
                            P.act(lambda e, q_=q_, pr=pr, c0=c0, src=src: e.activation(out=A[1][pr, q_:16:4, c0:c0 + 16], in_=src[pr, q_:16:4, :], func=AF.Copy), reads=[skey], writes=AK[1])

                def evac_b(t, b, i=i, c=c):
                    lo = 64 if t == 3 else 32 * t
                    hi = 128 if t == 3 else 32 * t + 32
                    P.act(lambda e, b=b: e.activation(out=LBk[i][lo:hi, c, :, :], in_=ps[b][lo:hi, :].rearrange("p (m n) -> p m n", m=4), func=AF.Copy),
                          reads=[psk(b)], writes=["LBk%d" % i])
                transposes(i, evac_b)

        L0 = [xflat[:, 0:512].rearrange("p (s c) -> p s c", s=16), xflat[:, 512:1024].rearrange("p (s c) -> p s c", s=16)]
        LT = [xflat[:, 1024:1536].rearrange("p (s c) -> p s c", s=16), xflat[:, 1536:2048].rearrange("p (s c) -> p s c", s=16)]
        for i in range(2):
            for q_ in range(4):
                for g2 in range(2):
                    j = 2 * q_ + g2
                    msk = cmk[:, 2 + i, j:j + 1]
                    P.dve(lambda e, i=i, q_=q_, g2=g2, msk=msk: e.tensor_scalar(
                        out=A[i][:, q_:16:4, g2 * 64:(g2 + 1) * 64], in0=cnat[i][:, :, :], scalar1=msk, scalar2=None, op0=ALU.mult),
                        reads=["cnat%d" % i, "cmk2", "cmk3"], writes=AK[i])

            def evac_c(t, b, i=i):
                P.act(lambda e, b=b, t=t: e.activation(out=L0[i][:, t:16:4, :], in_=ps[b][:, :].rearrange("p (m n) -> p m n", m=4)[:, :, 32 * t:32 * t + 32], func=AF.Copy),
                      reads=[psk(b)], writes=XK)
            transposes(i, evac_c)
        P.dve(lambda e: e.tensor_copy(out=LCk[0][:, 0, :, :], in_=L0[0]), reads=XK, writes=["LCk0"])
        P.dve(lambda e: e.tensor_copy(out=LCk[1][:, 0, :, :], in_=L0[1]), reads=XK, writes=["LCk1"])
        for c in range(1, 4):
            a_b = S(16 + 2 * c).unsqueeze(2).to_broadcast([128, 16, 32])
            b_b = S(17 + 2 * c).unsqueeze(2).to_broadcast([128, 16, 32])
            ak, bk = sk(16 + 2 * c), sk(17 + 2 * c)
            tt(LT[0], L0[0], a_b, ALU.mult, XK + [ak], XK)
            tt(LT[1], L0[1], b_b, ALU.mult, XK + [bk], XK)
            tt(LCk[0][:, c, :, :], LT[0], LT[1], ALU.add, XK, ["LCk0"])
            tt(LT[0], L0[1], a_b, ALU.mult, XK + [ak], XK)
            tt(LT[1], L0[0], b_b, ALU.mult, XK + [bk], XK)
            tt(LCk[1][:, c, :, :], LT[0], LT[1], ALU.subtract, XK, ["LCk1"])
        for m in range(4):
            P.dve(lambda e, m=m: e.tensor_scalar(out=diagD[:, m, :], in0=idf[:], scalar1=dcol[:, m:m + 1], scalar2=None, op0=ALU.mult),
                  reads=["idf", "cols"], writes=["diagD"])
        P.dve(lambda e: e.memset(wlast[:], 0.0), writes=[("wlast", s_) for s_ in range(16)])
        P.dve(lambda e: e.memset(cvbuf, 0.0), writes=CVK)

        def rms_to_T(gcol, gkey, final=False, hook=None):
            for j in range(4):
                P.act(lambda e, j=j: e.activation(out=junk, in_=xres[:, j, :], func=AF.Square, accum_out=stat[:, j:j + 1]),
                      reads=[("x", j)], writes=["ost", ("stat", j)])
            SK = [("stat", j) for j in range(4)]
            P.dve(lambda e: e.tensor_scalar(out=stat[:, 4:8], in0=stat[:, 0:4], scalar1=1.0 / D, scalar2=1e-6, op0=ALU.mult, op1=ALU.add),
                  reads=SK, writes=["stat2"])
            P.act(lambda e: e.activation(out=stat[:, 4:8], in_=stat[:, 4:8], func=AF.Sqrt), reads=["stat2"], writes=["stat2"])
            P.dve(lambda e: e.reciprocal(out=stat[:, 8:12], in_=stat[:, 4:8]), reads=["stat2"], writes=["rstd"])
            if final:
                return
            for j in range(4):
                P.act(lambda e, j=j: e.activation(out=hn[:, j % 2, :], in_=xres[:, j, :], func=AF.Copy, scale=stat[:, 8 + j:9 + j]),
                      reads=[("x", j), "rstd"], writes=[("hn", j % 2)])
                if hook is not None:
                    hook(j)
                b = PS.alloc()
                pv = ps[b][:].bitcast(BF16)
                for kt in range(8):
                    P.pe(lambda e, j=j, kt=kt, pv=pv: e.transpose(out=pv[:, kt * 128:(kt + 1) * 128], in_=hn[:, j % 2, kt * 128:(kt + 1) * 128], identity=idb[:]),
                         reads=[("hn", j % 2), "idb"], writes=[psk(b)])
                P.dve(lambda e, j=j, pv=pv: e.tensor_tensor(
                    out=hT[:, :, j * 128:(j + 1) * 128], in0=pv.rearrange("p (k t) -> p k t", k=8),
                    in1=gcol.unsqueeze(2).to_broadcast([128, 8, 128]), op=ALU.mult),
                    reads=[psk(b), gkey], writes=[("hT", j)])
                PS.free(b)

        HTK = [("hT", j) for j in range(4)]

        def win_tile(ct, hchunk):
            WS.check(hchunk)
            _, wv, wk = hchunk
            b = PS.alloc()
            o = (ct % 2) * 128
            for kt in range(8):
                P.pe(lambda e, kt=kt, b=b, o=o, wv=wv: e.matmul(ps[b][:], lhsT=wv[:, kt, o:o + 128], rhs=hT[:, kt, :], start=(kt == 0), stop=(kt == 7)),
                     reads=[wk] + HTK, writes=[psk(b)])
            return b

        def mm4(hchunk, mo, rhs_t, rkeys):
            WS.check(hchunk)
            _, wv, wk = hchunk
            b = PS.alloc()
            o = (mo % 4) * 128
            for kt in range(4):
                P.pe(lambda e, kt=kt, b=b, o=o, wv=wv: e.matmul(ps[b][:], lhsT=wv[:, kt, o:o + 128], rhs=rhs_t[:, kt, :], start=(kt == 0), stop=(kt == 3)),
                     reads=[wk] + rkeys, writes=[psk(b)])
            return b

        def mixer_block(blk):
            tok0 = blk * TB
            first = (blk * TB) % SEQ == 0
            for j in range(4):
                P.dma("sp", xres[:, j, :], x_d[tok0 + j * 128:tok0 + (j + 1) * 128, :], writes=[("x", j)], key=("x", j))
            rms_to_T(gmix, "cols")
            for m in range(4):
                if m % 2 == 0:
                    hc = WS.get(("win", m // 2))
                b = win_tile(m, hc)
                P.act(lambda e, m=m, b=b: e.activation(out=uT[:, m, :], in_=ps[b][:], func=AF.Identity, bias=binc[:, m:m + 1], scale=1.0),
                      reads=[psk(b), "cols"], writes=[("uT", m)])
                PS.free(b)
            if first:
                P.dve(lambda e: e.memset(wlast[:], 0.0), writes=[("wlast", s_) for s_ in range(16)])
                P.dve(lambda e: e.memset(cvbuf[:, :, 0:2], 0.0), writes=CVK)
            yb = None
            SETS = ((s5t, "s5t"), (s5u, "s5u"))
            for pr in range(8):
                m = pr // 2
                tiles = (2 * pr, 2 * pr + 1)
                banks = []
                for t, s in enumerate(tiles):
                    bxr = PS.alloc()
                    bxi = PS.alloc()
                    q = s % 4
                    kw = {"tile_position": (96, 0)} if q == 3 else {}
                    for c in range(4):
                        for i, bb in ((0, bxr), (1, bxi)):
                            P.pe(lambda e, q=q, m=m, c=c, i=i, b=bb, kw=kw: e.matmul(ps[b][:, c * 128:(c + 1) * 128], lhsT=LBk[i][32 * q:32 * q + 32, c, m, :],
                                                                                    rhs=uT[32 * q:32 * q + 32, m, c * 128:(c + 1) * 128], start=True, stop=True, **kw),
                                 reads=["LBk%d" % i, ("uT", m)], writes=[psk(bb)])
                    banks.append((bxr, bxi))

                def v3(ap):
                    return ap.rearrange("p (a c) -> p a c", a=4)
                ctx = []
                for t, s in enumerate(tiles):
                    buf, nm = SETS[t]
                    T = [buf[:, i, :] for i in range(4)]
                    TK = [(nm, i) for i in range(4)]
                    cosb = COS[:, s, :].unsqueeze(1).to_broadcast([128, 4, CH])
                    sinb = SIN[:, s, :].unsqueeze(1).to_broadcast([128, 4, CH])
                    ctx.append((s, T, TK, cosb, sinb, banks[t]))
                for step in range(6):
                    for (s, T, TK, cosb, sinb, (bxr, bxi)) in (ctx if step in (0, 1) else ctx[::-1]):
                        xr, xi = ps[bxr][:], ps[bxi][:]
                        if step == 0:
                            tt(v3(T[0]), v3(xr), cosb, ALU.mult, [psk(bxr), "COS"], [TK[0]])
                        elif step == 2:
                            tt(v3(T[1]), v3(xi), sinb, ALU.mult, [psk(bxi), "SIN"], [TK[1]])
                        elif step == 1:
                            tt(v3(T[2]), v3(xi), cosb, ALU.mult, [psk(bxi), "COS"], [TK[2]])
                        elif step == 3:
                            tt(v3(T[3]), v3(xr), sinb, ALU.mult, [psk(bxr), "SIN"], [TK[3]])
                        elif step == 4:
                            tt(T[0], T[0], T[1], ALU.add, [TK[0], TK[1]], [TK[0]])
                        else:
                            tt(T[2], T[2], T[3], ALU.subtract, [TK[2], TK[3]], [TK[2]])
                for (bxr, bxi) in banks:
                    PS.free(bxr)
                    PS.free(bxi)
                for step in range(4):
                    for t, (s, T, TK, cosb, sinb, _) in enumerate(ctx):
                        cE, sE = S(13)[:, s:s + 1], S(14)[:, s:s + 1]
                        rb = S(1)[:, s:s + 1].to_broadcast([128, TB])
                        lr, li = wlast[:, s, 0:1], wlast[:, s, 1:2]
                        lk = [("wlast", s)]
                        ck = [("chn", t, i) for i in range(4)]
                        if step == 0:
                            P.dve(lambda e, li=li, sE=sE, t=t: e.tensor_scalar(out=chn[:, t, 0:1], in0=li, scalar1=sE, scalar2=None, op0=ALU.mult), reads=lk + [sk(14)], writes=[ck[0]])
                            P.dve(lambda e, li=li, cE=cE, t=t: e.tensor_scalar(out=chn[:, t, 2:3], in0=li, scalar1=cE, scalar2=None, op0=ALU.mult), reads=lk + [sk(13)], writes=[ck[2]])
                        elif step == 1:
                            P.dve(lambda e, lr=lr, cE=cE, t=t: e.scalar_tensor_tensor(out=chn[:, t, 1:2], in0=lr, scalar=cE, in1=chn[:, t, 0:1], op0=ALU.mult, op1=ALU.subtract),
                                  reads=lk + [sk(13), ck[0]], writes=[ck[1]])
                            P.dve(lambda e, lr=lr, sE=sE, t=t: e.scalar_tensor_tensor(out=chn[:, t, 3:4], in0=lr, scalar=sE, in1=chn[:, t, 2:3], op0=ALU.mult, op1=ALU.add),
                                  reads=lk + [sk(14), ck[2]], writes=[ck[3]])
                        elif step == 2:
                            P.dve(lambda e, rb=rb, T=T, t=t: e.tensor_tensor_scan(out=T[1], data0=rb, data1=T[0], initial=chn[:, t, 1:2], op0=ALU.mult, op1=ALU.add),
                                  reads=[TK[0], ck[1], sk(1)], writes=[TK[1]])
                        else:
                            P.dve(lambda e, rb=rb, T=T, t=t: e.tensor_tensor_scan(out=T[3], data0=rb, data1=T[2], initial=chn[:, t, 3:4], op0=ALU.mult, op1=ALU.add),
                                  reads=[TK[2], ck[3], sk(1)], writes=[TK[3]])
                for t, (s, T, TK, cosb, sinb, _) in enumerate(ctx):
                    P.act(lambda e, s=s, T=T: e.activation(out=wlast[:, s, 0:1], in_=T[1][:, TB - 1:TB], func=AF.Copy), reads=[TK[1]], writes=[("wlast", s)])
                    P.act(lambda e, s=s, T=T: e.activation(out=wlast[:, s, 1:2], in_=T[3][:, TB - 1:TB], func=AF.Copy), reads=[TK[3]], writes=[("wlast", s)])
                for t, (s, T, TK, cosb, sinb, _) in enumerate(ctx):
                    sr_o, si_o = sri[:, t, 0, :], sri[:, t, 1, :]
                    SRK = [("sri", t, 0), ("sri", t, 1)]
                    Q0, Q1 = s5p[:, 0, :], s5p[:, 1, :]
                    QK = [("s5p", 0), ("s5p", 1)]

                    if t == 1:
                        Q0, Q1 = T[0], T[2]
                        QK = [TK[0], TK[2]]

                    def pt(out, a, b, op, reads, writes, t=t):
                        if t == 0:
                            P.pool(lambda e: e.tensor_tensor(out=out, in0=a, in1=b, op=op), reads=reads, writes=writes)
                        else:
                            P.dve(lambda e: e.tensor_tensor(out=out, in0=a, in1=b, op=op), reads=reads, writes=writes)
                    pt(v3(Q0), v3(T[1]), cosb, ALU.mult, [TK[1], "COS"], [QK[0]])
                    pt(v3(Q1), v3(T[3]), sinb, ALU.mult, [TK[3], "SIN"], [QK[1]])
                    pt(sr_o, Q0, Q1, ALU.subtract, QK, [SRK[0]])
                    pt(v3(Q0), v3(T[1]), sinb, ALU.mult, [TK[1], "SIN"], [QK[0]])
                    pt(v3(Q1), v3(T[3]), cosb, ALU.mult, [TK[3], "COS"], [QK[1]])
                    pt(si_o, Q0, Q1, ALU.add, QK, [SRK[1]])
                    q = s % 4
                    if q == 0:
                        yb = PS.alloc()
                    kw = {"tile_position": (0, 96)} if q == 3 else {}
                    for c in range(4):
                        for i, (src, skey) in enumerate(((sr_o, SRK[0]), (si_o, SRK[1]))):
                            first = (c == 0 and i == 0)
                            P.pe(lambda e, s=s, q=q, c=c, i=i, b=yb, src=src, kw=kw, first=first: e.matmul(
                                ps[b][32 * q:32 * q + 32, c * 128:(c + 1) * 128], lhsT=LCk[i][:, c, s, :], rhs=src[:, c * 128:(c + 1) * 128], start=first, stop=False, **kw),
                                reads=["LCk%d" % i, skey], writes=[psk(yb)])
                    if s % 4 == 3:
                        P.pe(lambda e, m=m, b=yb: e.matmul(ps[b][:], lhsT=diagD[:, m, :], rhs=uT[:, m, :], start=False, stop=True),
                             reads=["diagD", ("uT", m)], writes=[psk(yb)])
                        P.act(lambda e, m=m, b=yb: e.activation(out=ybf[:, m, :], in_=ps[b][:], func=AF.Gelu_apprx_tanh), reads=[psk(yb)], writes=[("ybf", m)])
                        PS.free(yb)
            emit_prepass(12)
            YBK = [("ybf", m) for m in range(4)]
            for m in range(4):
                if m % 2 == 0:
                    hc = WS.get(("win", 4 + m // 2))
                b = win_tile(8 + m, hc)
                P.act(lambda e, m=m, b=b: e.activation(out=abuf[:, m, :], in_=ps[b][:], func=AF.Identity, bias=binc[:, 8 + m:9 + m], scale=1.0),
                      reads=[psk(b), "cols"], writes=[("abuf", m)])
                PS.free(b)
            for m in range(4):
                if m % 2 == 0:
                    hc = WS.get(("win", 6 + m // 2))
                b = win_tile(12 + m, hc)
                P.dve(lambda e, m=m, b=b: e.scalar_tensor_tensor(out=cvbuf[:, m, 2:TB + 2], in0=ps[b][:], scalar=binc[:, 12 + m:13 + m], in1=abuf[:, m, :],
                                                                 op0=ALU.add, op1=ALU.mult), reads=[psk(b), "cols", ("abuf", m)], writes=[("cv", m)])
                PS.free(b)
                P.dve(lambda e, m=m: e.tensor_scalar(out=abuf[:, m, :], in0=cvbuf[:, m, 2:TB + 2], scalar1=cw[:, 8 + m:9 + m], scalar2=cbc[:, m:m + 1],
                                                     op0=ALU.mult, op1=ALU.add), reads=[("cv", m), "cols"], writes=[("abuf", m)])
                P.dve(lambda e, m=m: e.scalar_tensor_tensor(out=abuf[:, m, :], in0=cvbuf[:, m, 1:TB + 1], scalar=cw[:, 4 + m:5 + m], in1=abuf[:, m, :],
                                                            op0=ALU.mult, op1=ALU.add), reads=[("cv", m), "cols", ("abuf", m)], writes=[("abuf", m)])
                P.dve(lambda e, m=m: e.scalar_tensor_tensor(out=abuf[:, m, :], in0=cvbuf[:, m, 0:TB], scalar=cw[:, m:m + 1], in1=abuf[:, m, :],
                                                            op0=ALU.mult, op1=ALU.add), reads=[("cv", m), "cols", ("abuf", m)], writes=[("abuf", m)])
                P.act(lambda e, m=m: e.activation(out=cvbuf[:, m, 0:2], in_=cvbuf[:, m, TB:TB + 2], func=AF.Copy), reads=[("cv", m), ("abuf", m)], writes=[("cv", m)])
            for m in range(4):
                if m % 2 == 0:
                    hc = WS.get(("win", 2 + m // 2))
                b = win_tile(4 + m, hc)
                P.dve(lambda e, m=m, b=b: e.scalar_tensor_tensor(out=gbT[:, m, :], in0=ps[b][:], scalar=binc[:, 4 + m:5 + m], in1=abuf[:, m, :],
                                                                 op0=ALU.add, op1=ALU.mult), reads=[psk(b), "cols", ("abuf", m)], writes=[("gbT", m)])
                PS.free(b)
            GBK = [("gbT", m) for m in range(4)]
            TA, TBb, TC = s5t[:, 0, :], s5t[:, 1, :], s5t[:, 2, :]
            for mo in range(8):
                if mo % 4 == 0:
                    ha = WS.get(("glua", mo // 4))
                    hb = WS.get(("glub", mo // 4))
                if mo % 2 == 0:
                    hg = WS.get(("win", 8 + mo // 2))
                ba = mm4(ha, mo, ybf, YBK)
                bb = mm4(hb, mo, ybf, YBK)
                bg = win_tile(16 + mo, hg)
                P.act(lambda e, b=bb: e.activation(out=TA, in_=ps[b][:], func=AF.Sigmoid), reads=[psk(bb)], writes=[("s5t", 0)])
                P.act(lambda e, b=bg, mo=mo: e.activation(out=TBb, in_=ps[b][:], func=AF.Sigmoid, bias=binc[:, 16 + mo:17 + mo], scale=1.0),
                      reads=[psk(bg), "cols"], writes=[("s5t", 1)])
                tt(TA, ps[ba][:], TA, ALU.mult, [psk(ba), ("s5t", 0)], [("s5t", 0)])
                tt(mixT[:, mo, :], TA, TBb, ALU.mult, [("s5t", 0), ("s5t", 1)], [("mixT", mo)])
                PS.free(ba)
                PS.free(bb)
                PS.free(bg)
            for mo in range(8):
                if mo % 4 == 0:
                    hco = WS.get(("wco", mo // 4))
                if mo % 2 == 0:
                    hg = WS.get(("win", 12 + mo // 2))
                by = mm4(hco, mo, gbT, GBK)
                bg = win_tile(24 + mo, hg)
                P.act(lambda e, b=bg, mo=mo: e.activation(out=TC, in_=ps[b][:], func=AF.Sigmoid, bias=binc[:, 24 + mo:25 + mo], scale=1.0),
                      reads=[psk(bg), "cols"], writes=[("s5t", 2)])
                tt(TC, ps[by][:], TC, ALU.mult, [psk(by), ("s5t", 2)], [("s5t", 2)])
                tt(mixT[:, mo, :], mixT[:, mo, :], TC, ALU.add, [("mixT", mo), ("s5t", 2)], [("mixT", mo)])
                PS.free(by)
                PS.free(bg)
            MXK = [("mixT", mo) for mo in range(8)]
            for hf in range(2):
                h0 = WS.get(("wo", hf, 0))
                h1 = WS.get(("wo", hf, 1))
                for j in range(4):
                    b = PS.alloc()
                    for kt in range(8):
                        hcur = h0 if kt < 4 else h1
                        WS.check(hcur)
                        P.pe(lambda e, j=j, kt=kt, b=b, wv=hcur[1]: e.matmul(ps[b][:], lhsT=mixT[:, kt, j * 128:(j + 1) * 128], rhs=wv[:, kt % 4, :],
                                                                          start=(kt == 0), stop=(kt == 7)), reads=[hcur[2]] + MXK, writes=[psk(b)])
                    xs = xres[:, j, hf * 512:(hf + 1) * 512]
                    tt(xs, xs, ps[b][:], ALU.add, [("x", j), psk(b)], [("x", j)])
                    PS.free(b)

        NU = 96
        gffnB = P.sb("gffnB", [128, D], F32)
        ustr = P.sb("ustr", [128, 128], BF16)
        onesb = P.sb("onesb", [128, 128], BF16)
        m12b = P.sb("m12b", [128, 4, 32], BF16)
        rq = P.sb("rq", [128, 8, 16], F32)
        rw = P.sb("rw", [128, 5, 128], F32)
        m1s = P.sb("m1s", [128, 32, 32], BF16)
        m2s = P.sb("m2s", [128, 32, 32], BF16)
        srt = P.sb("srt", [128, 12, 32], F32)
        posi = P.sb("posi", [128, 2, 32], I32)
        uv = P.sb("uv", [128, 3, NU], F32)
        idxw = P.sb("idxw", [128, NU], I32)
        pidx = P.sb("pidx", [128, 1], F32)
        st3 = P.sb("st3", [128, 32], F32)
        junk3b = P.sb("junk3b", [128, D], BF16)
        h2b = P.sb("h2b", [128, 2, D], BF16)
        sgu = P.sb("sgu", [128, 2, 256], F32)
        hidU = P.sb("hidU", [128, 2, 2, 128], BF16)
        P.dma("sp", gffnB[:], norm_ffn.partition_broadcast(128), writes=["gffnB"], key="gffnB")
        P.dve(lambda e: e.tensor_single_scalar(out=ustr[:], in_=idi[:], scalar=0.0, op=ALU.is_gt), reads=["idi"], writes=["ustr"])
        P.dve(lambda e: e.memset(onesb[:], 1.0), writes=["onesb"])
        P.dve(lambda e: e.memset(srt[:], 0.0), writes=["srt"])
        P.dve(lambda e: e.memset(srt[:, 9, :], 1.0), reads=["srt"], writes=["srt9"])
        P.pool(lambda e: e.iota(srt[:, 5, :], pattern=[[128, 32]], base=0, channel_multiplier=0, allow_small_or_imprecise_dtypes=True), reads=["srt"], writes=["srt5"])
        P.pool(lambda e: e.iota(uv[:, 0, :], pattern=[[1, NU]], base=0, channel_multiplier=0, allow_small_or_imprecise_dtypes=True), writes=["uv0"])
        P.pool(lambda e: e.iota(pidx[:], pattern=[[0, 1]], base=0, channel_multiplier=1, allow_small_or_imprecise_dtypes=True), writes=["pidx"])
        P.dve(lambda e: e.memset(m1s[:], 0.0), writes=["m1s"])
        P.dve(lambda e: e.memset(m2s[:], 0.0), writes=["m2s"])
        carry = srt[:, 0, :]
        HSW = []
        YSK = [("ys", u) for u in range(NU)]

        def h2_hook(j, blk):
            J = blk * 4 + j
            P.dve(lambda e, j=j: e.tensor_tensor(out=h2b[:, j % 2, :], in0=hn[:, j % 2, :], in1=gffnB[:], op=ALU.mult),
                  reads=[("hn", j % 2), "gffnB"], writes=[("h2b", j % 2)])
            P.dma("sp", h2_s[J * 128:(J + 1) * 128, :], h2b[:, j % 2, :], reads=[("h2b", j % 2)], writes=[("h2s", J)], key=("h2b", j % 2))

        def route_block(blk):
            tok0 = blk * TB
            J0 = blk * 4
            for j in range(4):
                P.dma("sp", x1_s[tok0 + j * 128:tok0 + (j + 1) * 128, :], xres[:, j, :], reads=[("x", j)], writes=[("x1s", blk, j)], key=("x1st", j))
            rms_to_T(gffn, "cols", hook=lambda j: h2_hook(j, blk))
            b = PS.alloc()
            for j in range(4):
                for kt in range(8):
                    P.pe(lambda e, j=j, kt=kt, b=b: e.matmul(ps[b][:, j * 36:(j + 1) * 36], lhsT=hT[:, kt, j * 128:(j + 1) * 128], rhs=wr[:, kt, :], start=(kt == 0), stop=(kt == 7)),
                         reads=HTK + WRK, writes=[psk(b)])
            P.dve(lambda e, b=b: e.tensor_tensor(out=lsb[:], in0=ps[b][:, 0:144].rearrange("p (j n) -> p j n", j=4), in1=brB[:].unsqueeze(1).to_broadcast([128, 4, 36]), op=ALU.add),
                  reads=[psk(b)] + BRB, writes=["lsb"])
            PS.free(b)
            lg = lsb[:, :, 0:4]
            le4 = lsb[:, :, 4:36].rearrange("p j (g k) -> p j g k", g=4)
            q4 = lambda i: rq[:, i, 0:4]
            q16 = lambda i: rq[:, i, 0:16].rearrange("p (j g) -> p j g", j=4)
            W3 = lambda i: rw[:, i, :].rearrange("p (j n) -> p j n", j=4)
            bc4 = lambda ap: ap.unsqueeze(2).to_broadcast([128, 4, 4])
            bc32 = lambda ap: ap.unsqueeze(2).to_broadcast([128, 4, 32])
            P.dve(lambda e: e.tensor_reduce(out=q4(0), in_=lg, axis=AX.X, op=ALU.max), reads=["lsb"], writes=["q0"])
            P.dve(lambda e: e.tensor_tensor(out=q16(1), in0=lg, in1=bc4(q4(0)), op=ALU.is_ge), reads=["lsb", "q0"], writes=["q1"])
            P.dve(lambda e: e.tensor_scalar(out=rq[:, 2, 0:16], in0=rq[:, 1, 0:16], scalar1=BIG, scalar2=-BIG, op0=ALU.mult, op1=ALU.add), reads=["q1"], writes=["q2"])
            P.dve(lambda e: e.tensor_tensor(out=q16(3), in0=lg, in1=bc4(q4(0)), op=ALU.subtract), reads=["lsb", "q0"], writes=["q3"])
            P.act(lambda e: e.activation(out=rq[:, 3, 0:16], in_=rq[:, 3, 0:16], func=AF.Exp), reads=["q3"], writes=["q3"])
            P.dve(lambda e: e.tensor_reduce(out=q4(4), in_=q16(3), axis=AX.X, op=ALU.add), reads=["q3"], writes=["q4"])
            P.dve(lambda e: e.reciprocal(out=q4(4), in_=q4(4)), reads=["q4"], writes=["q4"])
            P.dve(lambda e: e.tensor_tensor(out=rw[:, 0, :].rearrange("p (j g k) -> p j g k", j=4, g=4), in0=le4,
                                            in1=q16(2).unsqueeze(3).to_broadcast([128, 4, 4, 8]), op=ALU.add), reads=["lsb", "q2"], writes=["w0"])
            P.dve(lambda e: e.tensor_reduce(out=q4(5), in_=W3(0), axis=AX.X, op=ALU.max), reads=["w0"], writes=["q5"])
            P.dve(lambda e: e.tensor_tensor(out=W3(1), in0=W3(0), in1=bc32(q4(5)), op=ALU.is_ge), reads=["w0", "q5"], writes=["w1"])
            P.dve(lambda e: e.scalar_tensor_tensor(out=W3(2), in0=W3(1), scalar=-BIG, in1=W3(0), op0=ALU.mult, op1=ALU.add), reads=["w1", "w0"], writes=["w2"])
            P.dve(lambda e: e.tensor_reduce(out=q4(6), in_=W3(2), axis=AX.X, op=ALU.max), reads=["w2"], writes=["q6"])
            P.dve(lambda e: e.tensor_tensor(out=W3(3), in0=W3(0), in1=bc32(q4(6)), op=ALU.is_ge), reads=["w0", "q6"], writes=["w3"])
            P.dve(lambda e: e.tensor_copy(out=m12b[:], in_=W3(3)), reads=["w3"], writes=["m12b"])
            P.dve(lambda e: e.tensor_copy(out=m1s[:, J0:J0 + 4, :], in_=W3(1)), reads=["w1", "m1s"], writes=[("m1s", J0)])
            P.dve(lambda e: e.tensor_tensor(out=m2s[:, J0:J0 + 4, :], in0=W3(3), in1=W3(1), op=ALU.subtract), reads=["w3", "w1", "m2s"], writes=[("m2s", J0)])
            P.dve(lambda e: e.tensor_tensor(out=q4(7), in0=q4(5), in1=q4(6), op=ALU.add), reads=["q5", "q6"], writes=["q7"])
            P.dve(lambda e: e.tensor_scalar(out=rw[:, 2, :], in0=rw[:, 0, :], scalar1=2.0, scalar2=None, op0=ALU.mult), reads=["w0", "w2"], writes=["w2"])
            P.dve(lambda e: e.tensor_tensor(out=W3(2), in0=W3(2), in1=bc32(q4(7)), op=ALU.subtract), reads=["w2", "q7"], writes=["w2"])
            P.act(lambda e: e.activation(out=rw[:, 2, :], in_=rw[:, 2, :], func=AF.Sigmoid), reads=["w2"], writes=["w2"])
            P.dve(lambda e: e.tensor_tensor(out=W3(2), in0=W3(2), in1=bc32(q4(4)), op=ALU.mult), reads=["w2", "q4"], writes=["w2"])
            P.dve(lambda e: e.tensor_tensor(out=W3(2), in0=W3(2), in1=W3(3), op=ALU.mult), reads=["w2", "w3"], writes=["w2"])
            b = PS.alloc()
            for j in range(4):
                P.pe(lambda e, b=b, j=j: e.matmul(ps[b][:, j * 64:j * 64 + 32], lhsT=ustr[:], rhs=m12b[:, j, :], start=True, stop=True), reads=["ustr", "m12b"], writes=[psk(b)])
                P.pe(lambda e, b=b, j=j: e.matmul(ps[b][:, j * 64 + 32:j * 64 + 64], lhsT=onesb[:], rhs=m12b[:, j, :], start=True, stop=True), reads=["onesb", "m12b"], writes=[psk(b)])
            for j in range(4):
                tt(rw[:, 4, j * 32:(j + 1) * 32], ps[b][:, j * 64:j * 64 + 32], carry, ALU.add, [psk(b), "srt", "carry"], [("w4", j)])
                tt(carry, carry, ps[b][:, j * 64 + 32:j * 64 + 64], ALU.add, [psk(b), "srt", "carry", ("w4", j)], ["carry"])
            PS.free(b)
            W4K = [("w4", j) for j in range(4)]
            P.dve(lambda e: e.tensor_tensor(out=W3(0), in0=W3(3), in1=W3(1), op=ALU.subtract), reads=["w3", "w1", "w2"], writes=["w0"])
            for (msk, mk, col_r, col_w) in ((W3(1), "w1", 1, 3), (W3(0), "w0", 2, 4)):
                P.dve(lambda e, msk=msk: e.tensor_tensor(out=W3(3), in0=msk, in1=W3(4), op=ALU.mult), reads=[mk, "w0"] + W4K, writes=["w3"])
                P.dve(lambda e, col_r=col_r: e.tensor_reduce(out=srt[:, col_r, J0:J0 + 4], in_=W3(3), axis=AX.X, op=ALU.add), reads=["w3", "srt"], writes=[("srtc", col_r, J0)])
                P.dve(lambda e, msk=msk: e.tensor_tensor(out=W3(3), in0=msk, in1=W3(2), op=ALU.mult), reads=[mk, "w2"], writes=["w3"])
                P.dve(lambda e, col_w=col_w: e.tensor_reduce(out=srt[:, col_w, J0:J0 + 4], in_=W3(3), axis=AX.X, op=ALU.add), reads=["w3", "srt"], writes=[("srtc", col_w, J0)])

        NJ = nblk * 4
        SRTC = [("srtc", c, 4 * b_) for c in (1, 2, 3, 4) for b_ in range(nblk)]

        def sort_phase():
            cmp3 = xflat[:, 0:1024].rearrange("p (a b) -> p a b", a=32)
            tiles, endc, base, ones32 = srt[:, 6, :], srt[:, 7, :], srt[:, 8, :], srt[:, 9, :]
            P.dve(lambda e: e.tensor_tensor(out=cmp3, in0=carry.unsqueeze(2).to_broadcast([128, 32, 32]), in1=srt[:, 5, :].unsqueeze(1).to_broadcast([128, 32, 32]), op=ALU.is_gt),
                  reads=["carry", "srt5"], writes=XK)
            P.dve(lambda e: e.tensor_reduce(out=tiles, in_=cmp3, axis=AX.X, op=ALU.add), reads=XK + ["srt"], writes=["srt6"])
            P.dve(lambda e: e.tensor_tensor_scan(out=endc, data0=ones32, data1=tiles, initial=0.0, op0=ALU.mult, op1=ALU.add), reads=["srt6", "srt9"], writes=["srt7"])
            tt(base, endc, tiles, ALU.subtract, ["srt6", "srt7"], ["srt8"])
            P.dve(lambda e: e.tensor_scalar(out=base, in0=base, scalar1=128.0, scalar2=None, op0=ALU.mult), reads=["srt8"], writes=["srt8"])
            for k, ms, mall in ((0, m1s, "m1s"), (1, m2s, "m2s")):
                MK = [(mall, 4 * b_) for b_ in range(nblk)] + [mall]
                P.dve(lambda e, ms=ms: e.tensor_tensor(out=cmp3, in0=ms[:], in1=base.unsqueeze(1).to_broadcast([128, 32, 32]), op=ALU.mult), reads=MK + ["srt8"], writes=XK)
                P.dve(lambda e, k=k: e.tensor_reduce(out=srt[:, 10 + k, :], in_=cmp3, axis=AX.X, op=ALU.add), reads=XK, writes=[("srt1x", k)])
                P.dve(lambda e, k=k: e.tensor_tensor(out=srt[:, 10 + k, :], in0=srt[:, 10 + k, :], in1=srt[:, 1 + k, :], op=ALU.add), reads=[("srt1x", k)] + SRTC, writes=[("srt1x", k)])
                P.dve(lambda e, k=k: e.tensor_copy(out=posi[:, k, :], in_=srt[:, 10 + k, :]), reads=[("srt1x", k)], writes=[("posi", k)])
            cmpu = xflat[:, 0:NU * 32].rearrange("p (a b) -> p a b", a=NU)
            P.dve(lambda e: e.tensor_tensor(out=cmpu, in0=uv[:, 0, :].unsqueeze(2).to_broadcast([128, NU, 32]), in1=endc.unsqueeze(1).to_broadcast([128, NU, 32]), op=ALU.is_ge),
                  reads=["uv0", "srt7"], writes=XK)
            P.dve(lambda e: e.tensor_reduce(out=uv[:, 1, :], in_=cmpu, axis=AX.X, op=ALU.add), reads=XK, writes=["uv1"])
            P.dve(lambda e: e.tensor_scalar(out=uv[:, 1, :], in0=uv[:, 1, :], scalar1=31.0, scalar2=None, op0=ALU.min), reads=["uv1"], writes=["uv1"])
            P.dve(lambda e: e.tensor_scalar(out=uv[:, 2, :], in0=uv[:, 1, :], scalar1=128.0, scalar2=pidx[:, 0:1], op0=ALU.mult, op1=ALU.add), reads=["uv1", "pidx"], writes=["uv2"])
            P.dve(lambda e: e.tensor_copy(out=idxw[:], in_=uv[:, 2, :]), reads=["uv2"], writes=["idxw"])

        def scatter_phase():
            hsb = mixT[:].rearrange("p a b -> p (a b)")[:, 0:2048].rearrange("p (r c) -> p r c", r=2)
            for J in range(NJ):
                par = J % 2
                hk = [("mixT", 2 * par), ("mixT", 2 * par + 1)]
                P.dma("sp", hsb[:, par, :], h2_s[J * 128:(J + 1) * 128, :], reads=[("h2s", J)], writes=hk, key=("hsb", par))
                for k in range(2):
                    key = ("hsw", J, k)
                    HSW.append(key)
                    P.add("pool", lambda e, J=J, k=k, par=par: e.indirect_dma_start(
                        out=hs, out_offset=bass.IndirectOffsetOnAxis(ap=posi[:, k, J:J + 1], axis=0), in_=hsb[:, par, :], in_offset=None),
                        reads=hk + [("posi", k)], writes=[key], dma_key=("hsc", par, k))

        arenaF = arena[:]
        s5f = s5t[:].rearrange("p a b -> p (a b)")
        def flatb(t):
            return t[:].rearrange("p a b -> p (a b)")
        WBUF = [(arenaF[:, 0:1024].bitcast(BF16), [("abuf", 0), ("abuf", 1)]), (arenaF[:, 1024:2048].bitcast(BF16), [("abuf", 2), ("abuf", 3)]),
                (arenaF[:, 2048:3072].bitcast(BF16), [("cv", 0), ("cv", 1)]), (arenaF[:, 3072:4096].bitcast(BF16), [("cv", 1), ("cv", 2), ("cv", 3)]),
                (s5f[:, 0:1024].bitcast(BF16), [("s5t", 0), ("s5t", 1)]), (s5f[:, 1024:2048].bitcast(BF16), [("s5t", 2), ("s5t", 3)]),
                (flatb(ybf), [("ybf", m) for m in range(4)]), (flatb(gbT), [("gbT", m) for m in range(4)]), (flatb(uT), [("uT", m) for m in range(4)])]

        def unit_phase():
            hsbT = mixT[:].rearrange("p a b -> p (a b)")[:, 2048:4096].rearrange("p (r k c) -> p r k c", r=2, k=8)
            hsl = mixT[:].rearrange("p a b -> p (a b)")[:, 0:2048].rearrange("p (r c) -> p r c", r=2)
            yo = xres[:].rearrange("p a b -> p (a b)").rearrange("p (r c) -> p r c", r=4)
            state = {}

            wsets = {}

            def gathers(u):
                st = u % 3
                wv = []
                for i, wsrc in enumerate((wgp, wup, wdp)):
                    ap, keys = WBUF[3 * st + i]
                    P.add("pool", lambda e, ap=ap, wsrc=wsrc, u=u: e.indirect_dma_start(
                        out=ap, out_offset=None, in_=wsrc, in_offset=bass.IndirectOffsetOnAxis(ap=idxw[:, u:u + 1], axis=0)),
                        reads=["idxw"] + WPK[i], writes=keys, dma_key=("wbuf", st, i))
                    wv.append((ap, keys))
                wsets[u] = wv

            def front(u):
                par = u % 2
                hk = [("mixT", 2 * par), ("mixT", 2 * par + 1)]
                tk = [("mixT", 4 + 2 * par), ("mixT", 5 + 2 * par)]
                if u == 0:
                    P.dma("sp", hsl[:, 0, :], hs[0:128, :], reads=HSW, writes=hk, key=("hsl", 0))
                if u + 1 < NU:
                    p1 = (u + 1) % 2
                    P.dma("sp", hsl[:, p1, :], hs[(u + 1) * 128:(u + 2) * 128, :], reads=HSW, writes=[("mixT", 2 * p1), ("mixT", 2 * p1 + 1)], key=("hsl", p1))
                wv = wsets.pop(u)
                b = PS.alloc()
                pv = ps[b][:].bitcast(BF16)
                for kt in range(8):
                    P.pe(lambda e, kt=kt, pv=pv, par=par: e.transpose(out=pv[:, kt * 128:(kt + 1) * 128], in_=hsl[:, par, kt * 128:(kt + 1) * 128], identity=idb[:]),
                         reads=hk + ["idb"], writes=[psk(b)])
                P.act(lambda e, pv=pv, par=par: e.activation(out=hsbT[:, par, :, :].rearrange("p k c -> p (k c)"), in_=pv, func=AF.Copy), reads=[psk(b)], writes=tk)
                PS.free(b)
                b = PS.alloc()
                for gi, (ap, keys) in enumerate(wv[0:2]):
                    w3 = ap.rearrange("p (k c) -> p k c", k=8)
                    for ht in range(2):
                        o = gi * 256 + ht * 128
                        for kt in range(8):
                            P.pe(lambda e, kt=kt, b=b, o=o, w3=w3, ht=ht, par=par: e.matmul(ps[b][:, o:o + 128], lhsT=w3[:, kt, ht * 128:(ht + 1) * 128], rhs=hsbT[:, par, kt, :],
                                                                                         start=(kt == 0), stop=(kt == 7)), reads=keys + tk, writes=[psk(b)])
                P.act(lambda e, b=b, par=par: e.activation(out=sgu[:, par, :], in_=ps[b][:, 0:256], func=AF.Silu), reads=[psk(b)], writes=[("sgu", par)])
                P.dve(lambda e, b=b, par=par: e.tensor_tensor(out=hidU[:, par, :, :].rearrange("p a b -> p (a b)"), in0=ps[b][:, 256:512], in1=sgu[:, par, :], op=ALU.mult),
                      reads=[psk(b), ("sgu", par)], writes=[("hidU", par)])
                PS.free(b)
                state[u] = wv[2]

            def back(u):
                par = u % 2
                q = u % 4
                ap, keys = state.pop(u)
                w3 = ap.rearrange("p (k c) -> p k c", k=2)
                yk = [("x", q)]
                for hf in range(2):
                    b = PS.alloc()
                    for ht in range(2):
                        P.pe(lambda e, hf=hf, ht=ht, b=b, w3=w3, par=par: e.matmul(ps[b][:], lhsT=hidU[:, par, ht, :], rhs=w3[:, ht, hf * 512:(hf + 1) * 512], start=(ht == 0), stop=(ht == 1)),
                             reads=keys + [("hidU", par)], writes=[psk(b)])
                    dst = xres[:, q, hf * 512:(hf + 1) * 512]
                    if hf == 0:
                        P.act(lambda e, b=b, dst=dst: e.activation(out=dst, in_=ps[b][:], func=AF.Copy), reads=[psk(b)], writes=yk)
                    else:
                        P.dve(lambda e, b=b, dst=dst: e.tensor_copy(out=dst, in_=ps[b][:]), reads=[psk(b)], writes=yk)
                    PS.free(b)
                P.dma("sp", ys[u * 128:(u + 1) * 128, :], xres[:, q, :], reads=yk, writes=[("ys", u)], key=("yst", q))

            gathers(0)
            gathers(1)
            for u in range(NU + 1):
                if u < NU:
                    front(u)
                if u >= 1:
                    back(u - 1)
                if u + 2 < NU:
                    gathers(u + 2)

        def combine_block(blk):
            tok0 = blk * TB
            yg = arena[:, 0:4096].rearrange("p (r c) -> p r c", r=4)
            for j in range(4):
                J = blk * 4 + j
                P.dma("sp", xres[:, j, :], x1_s[tok0 + j * 128:tok0 + (j + 1) * 128, :], reads=[("x1s", blk, j)], writes=[("x", j)], key=("x", j))
                for k in range(2):
                    r = (2 * j + k) % 4
                    gk = [[("abuf", 0), ("abuf", 1)], [("abuf", 2), ("abuf", 3)], [("cv", 0), ("cv", 1)], [("cv", 1), ("cv", 2), ("cv", 3)]][r]
                    P.add("pool", lambda e, r=r, k=k, J=J: e.indirect_dma_start(
                        out=yg[:, r, :], out_offset=None, in_=ys, in_offset=bass.IndirectOffsetOnAxis(ap=posi[:, k, J:J + 1], axis=0)),
                        reads=YSK + [("posi", k)], writes=gk, dma_key=("yg", r))
                    P.dve(lambda e, j=j, r=r, k=k, J=J: e.scalar_tensor_tensor(out=xres[:, j, :], in0=yg[:, r, :], scalar=srt[:, 3 + k, J:J + 1], in1=xres[:, j, :], op0=ALU.mult, op1=ALU.add),
                          reads=gk + [("x", j)] + SRTC, writes=[("x", j)])

        def ple_final(blk):
            tok0 = blk * TB
            rms_to_T(gple, "cols")
            P.dma("pool", pb[:], p_d[tok0:tok0 + TB, :].rearrange("(j p) c -> p j c", p=128), writes=["pb"], key="pb")
            b = PS.alloc()
            pv = ps[b][:].bitcast(BF16)
            for kk in range(2):
                for j in range(4):
                    P.pe(lambda e, kk=kk, j=j, pv=pv: e.transpose(out=pv[:, kk * 512 + j * 128:kk * 512 + (j + 1) * 128], in_=pb[:, j, kk * 128:(kk + 1) * 128], identity=idb[:]),
                         reads=["pb", "idb"], writes=[psk(b)])
            P.act(lambda e, pv=pv: e.activation(out=pT[:].rearrange("p a b -> p (a b)"), in_=pv, func=AF.Copy), reads=[psk(b)], writes=["pT"])
            PS.free(b)
            hple = None
            for hf in range(2):
                h0 = WS.get(("wpg", hf, 0))
                h1 = WS.get(("wpg", hf, 1))
                if hf == 0:
                    hple = WS.get(("wple",))
                for j in range(4):
                    b = PS.alloc()
                    for kt in range(8):
                        hcur = h0 if kt < 4 else h1
                        WS.check(hcur)
                        P.pe(lambda e, j=j, kt=kt, b=b, wv=hcur[1]: e.matmul(ps[b][:], lhsT=hT[:, kt, j * 128:(j + 1) * 128], rhs=wv[:, kt % 4, :],
                                                                          start=(kt == 0), stop=(kt == 7)), reads=[hcur[2]] + HTK, writes=[psk(b)])
                    WS.check(hple)
                    b2 = PS.alloc()
                    for kk in range(2):
                        P.pe(lambda e, j=j, hf=hf, kk=kk, b=b2, wv=hple[1]: e.matmul(ps[b][:], lhsT=pT[:, kk, j * 128:(j + 1) * 128], rhs=wv[:, kk, hf * 512:(hf + 1) * 512],
                                                                                  start=(kk == 0), stop=(kk == 1)), reads=[hple[2], "pT"], writes=[psk(b2)])
                    g = s5p[:, j % 2, :]
                    gk = ("s5p", j % 2)
                    tt(g, ps[b][:], bpgB[:, hf * 512:(hf + 1) * 512], ALU.add, [psk(b), "bpgB"], [gk])
                    P.act(lambda e, g=g: e.activation(out=g, in_=g, func=AF.Sigmoid), reads=[gk], writes=[gk])
                    tt(g, g, ps[b2][:], ALU.mult, [gk, psk(b2)], [gk])
                    xs = xres[:, j, hf * 512:(hf + 1) * 512]
                    tt(xs, xs, g, ALU.add, [("x", j), gk], [("x", j)])
                    PS.free(b)
                    PS.free(b2)
            rms_to_T(None, None, final=True)
            for j in range(4):
                P.dve(lambda e, j=j: e.scalar_tensor_tensor(out=ost[:], in0=xres[:, j, :], scalar=stat[:, 8 + j:9 + j], in1=gfinB[:], op0=ALU.mult, op1=ALU.mult),
                      reads=[("x", j), "rstd", "gfinB"], writes=["ost"])
                P.dma("sp", y_d[tok0 + j * 128:tok0 + (j + 1) * 128, :], ost[:], reads=["ost"], writes=[("y", j)], key=("y", j))


        def phase3():
            yg = arena[:, 0:4096].rearrange("p (r c) -> p r c", r=4)
            YGK = [[("abuf", 0), ("abuf", 1)], [("abuf", 2), ("abuf", 3)], [("cv", 0), ("cv", 1)], [("cv", 1), ("cv", 2), ("cv", 3)]]
            osts = [(ost[:], "ost"), (h2b[:, 0, :].bitcast(F32)[:, 0:512], None)]
            ost2 = h2b[:].rearrange("p a b -> p (a b)").bitcast(F32)
            osts = [(ost[:], ["ost"]), (ost2, [("h2b", 0), ("h2b", 1)])]
            hw = {}
            hw[(0, 0)] = WS.get(("wpg", 0, 0))
            hw[(0, 1)] = WS.get(("wpg", 0, 1))
            hple = WS.get(("wple",))
            hw[(1, 0)] = WS.get(("wpg", 1, 0))
            hw[(1, 1)] = WS.get(("wpg", 1, 1))

            def L(J):
                j = J % 4
                P.dma("sp", xres[:, j, :], x1_s[J * 128:(J + 1) * 128, :], reads=[("x1s", J // 4, j)], writes=[("x", j)], key=("x", j))
                for k in range(2):
                    r = (2 * J + k) % 4
                    P.add("pool", lambda e, r=r, k=k, J=J: e.indirect_dma_start(
                        out=yg[:, r, :], out_offset=None, in_=ys, in_offset=bass.IndirectOffsetOnAxis(ap=posi[:, k, J:J + 1], axis=0)),
                        reads=YSK + [("posi", k)], writes=YGK[r], dma_key=("yg", r))
                P.dma("pool", pb[:, j, :], p_d[J * 128:(J + 1) * 128, :], writes=[("pb", j)], key=("pb", j))

            def C(J):
                j = J % 4
                for k in range(2):
                    r = (2 * J + k) % 4
                    P.dve(lambda e, j=j, r=r, k=k, J=J: e.scalar_tensor_tensor(out=xres[:, j, :], in0=yg[:, r, :], scalar=srt[:, 3 + k, J:J + 1], in1=xres[:, j, :], op0=ALU.mult, op1=ALU.add),
                          reads=YGK[r] + [("x", j)] + SRTC, writes=[("x", j)])

            def rstd_tile(j, c0):
                P.act(lambda e, j=j: e.activation(out=junk3b[:], in_=xres[:, j, :], func=AF.Square, accum_out=st3[:, c0 + j:c0 + j + 1]),
                      reads=[("x", j)], writes=["junk3"] + [("st3", c0, j)])
                P.dve(lambda e, j=j: e.tensor_scalar(out=st3[:, c0 + 4 + j:c0 + 5 + j], in0=st3[:, c0 + j:c0 + j + 1], scalar1=1.0 / D, scalar2=1e-6, op0=ALU.mult, op1=ALU.add),
                      reads=[("st3", c0, j)], writes=[("st3b", c0, j)])
                P.act(lambda e, j=j: e.activation(out=st3[:, c0 + 4 + j:c0 + 5 + j], in_=st3[:, c0 + 4 + j:c0 + 5 + j], func=AF.Sqrt), reads=[("st3b", c0, j)], writes=[("st3b", c0, j)])
                P.dve(lambda e, j=j: e.reciprocal(out=st3[:, c0 + 8 + j:c0 + 9 + j], in_=st3[:, c0 + 4 + j:c0 + 5 + j]), reads=[("st3b", c0, j)], writes=[("st3r", c0, j)])

            def N(J):
                j = J % 4
                rstd_tile(j, 0)
                P.act(lambda e, j=j: e.activation(out=hn[:, j % 2, :], in_=xres[:, j, :], func=AF.Copy, scale=st3[:, 8 + j:9 + j]),
                      reads=[("x", j), ("st3r", 0, j)], writes=[("hn", j % 2)])
                b = PS.alloc()
                pv = ps[b][:].bitcast(BF16)
                for kt in range(8):
                    P.pe(lambda e, j=j, kt=kt, pv=pv: e.transpose(out=pv[:, kt * 128:(kt + 1) * 128], in_=hn[:, j % 2, kt * 128:(kt + 1) * 128], identity=idb[:]),
                         reads=[("hn", j % 2), "idb"], writes=[psk(b)])
                P.dve(lambda e, j=j, pv=pv: e.tensor_tensor(
                    out=hT[:, :, j * 128:(j + 1) * 128], in0=pv.rearrange("p (k t) -> p k t", k=8),
                    in1=gple.unsqueeze(2).to_broadcast([128, 8, 128]), op=ALU.mult),
                    reads=[psk(b), "cols"], writes=[("hT", j)])
                PS.free(b)
                b = PS.alloc()
                pv = ps[b][:].bitcast(BF16)
                for kk in range(2):
                    P.pe(lambda e, kk=kk, j=j, pv=pv: e.transpose(out=pv[:, kk * 128:(kk + 1) * 128], in_=pb[:, j, kk * 128:(kk + 1) * 128], identity=idb[:]),
                         reads=[("pb", j), "idb"], writes=[psk(b)])
                P.act(lambda e, pv=pv, j=j: e.activation(out=pT[:, :, j * 128:(j + 1) * 128], in_=pv[:, 0:256].rearrange("p (k t) -> p k t", k=2), func=AF.Copy),
                      reads=[psk(b)], writes=[("pT", j)])
                PS.free(b)

            def G(J):
                j = J % 4
                for hf in range(2):
                    b = PS.alloc()
                    for kt in range(8):
                        hcur = hw[(hf, kt // 4)]
                        P.pe(lambda e, j=j, kt=kt, b=b, wv=hcur[1]: e.matmul(ps[b][:], lhsT=hT[:, kt, j * 128:(j + 1) * 128], rhs=wv[:, kt % 4, :],
                                                                          start=(kt == 0), stop=(kt == 7)), reads=[hcur[2], ("hT", j)], writes=[psk(b)])
                    b2 = PS.alloc()
                    for kk in range(2):
                        P.pe(lambda e, j=j, hf=hf, kk=kk, b=b2, wv=hple[1]: e.matmul(ps[b][:], lhsT=pT[:, kk, j * 128:(j + 1) * 128], rhs=wv[:, kk, hf * 512:(hf + 1) * 512],
                                                                                  start=(kk == 0), stop=(kk == 1)), reads=[hple[2], ("pT", j)], writes=[psk(b2)])
                    g = s5p[:, hf, :]
                    gk = ("s5p", hf)
                    tt(g, ps[b][:], bpgB[:, hf * 512:(hf + 1) * 512], ALU.add, [psk(b), "bpgB"], [gk])
                    P.act(lambda e, g=g: e.activation(out=g, in_=g, func=AF.Sigmoid), reads=[gk], writes=[gk])
                    tt(g, g, ps[b2][:], ALU.mult, [gk, psk(b2)], [gk])
                    xs = xres[:, j, hf * 512:(hf + 1) * 512]
                    tt(xs, xs, g, ALU.add, [("x", j), gk], [("x", j)])
                    PS.free(b)
                    PS.free(b2)

            def Fin(J):
                j = J % 4
                rstd_tile(j, 16)
                oap, okeys = osts[J % 2]
                P.dve(lambda e, j=j, oap=oap: e.scalar_tensor_tensor(out=oap, in0=xres[:, j, :], scalar=st3[:, 24 + j:25 + j], in1=gfinB[:], op0=ALU.mult, op1=ALU.mult),
                      reads=[("x", j), ("st3r", 16, j), "gfinB"], writes=okeys)
                P.dma("sp", y_d[J * 128:(J + 1) * 128, :], oap, reads=okeys, writes=[("y", J % 4)], key=("y", J % 4))

            for it in range(NJ + 3):
                if 0 <= it - 2 < NJ:
                    C(it - 2)
                if it < NJ:
                    L(it)
                if 0 <= it - 3 < NJ:
                    G(it - 3)
                if 0 <= it - 2 < NJ:
                    N(it - 2)
                if 0 <= it - 3 < NJ:
                    Fin(it - 3)

        for blk in range(nblk):
            mixer_block(blk)
            route_block(blk)
        emit_prepass(1000)
        sort_phase()
        scatter_phase()
        unit_phase()
        phase3()

        P.add("sp", None, reads=[("y", j) for j in range(4)])
        P.finalize()
        info = dict(counts=P.counts, nsem=P.nsem, maxcnt=P.maxcnt)
    return nc, info


_WNAMES = ["norm_mix", "w_in", "b_in", "ssm_lam_re", "ssm_lam_im", "ssm_log_dt", "ssm_b_re", "ssm_b_im", "ssm_c_re",
           "ssm_c_im", "ssm_d", "w_glu_a", "w_glu_b", "conv_w", "conv_b", "w_conv_out", "w_o", "norm_ffn",
           "w_router_group", "b_router_group", "w_router_expert", "b_router_expert", "w_exp_gate", "w_exp_up",
           "w_exp_down", "norm_ple", "w_ple", "w_ple_gate", "b_ple_gate"]


def make_in_maps(inputs, ncores=NCORES):
    x = np.ascontiguousarray(np.asarray(inputs["x"], dtype=np.float32)).reshape(-1, D)
    p = np.ascontiguousarray(np.asarray(inputs["p"], dtype=np.float32)).reshape(-1, 256)
    shared = {k: np.ascontiguousarray(np.asarray(inputs[k], dtype=np.float32)[0]) for k in _WNAMES}
    shared["norm_final"] = np.ascontiguousarray(np.asarray(inputs["norm_final"], dtype=np.float32))
    maps = []
    for c in range(ncores):
        m = dict(shared)
        m["x"] = x[c * TOK:(c + 1) * TOK]
        m["p"] = p[c * TOK:(c + 1) * TOK]
        maps.append(m)
    return maps


def kernel(**inputs):
    nc, _ = build_program()
    in_maps = make_in_maps(inputs)
    res = run_bass_kernel_spmd(nc, in_maps, core_ids=list(range(NCORES)))
    out = np.concatenate([np.asarray(r["y"]) for r in res.results], axis=0)
    return out.reshape(16, SEQ, D).astype(np.float32)
```
